# Optimizing a Trainium2 kernel written in Bass

```python
import jax
import jax.numpy as jnp
from jax import lax
import numpy as np

D_MODEL = 1024
BATCH = 8
SEQ = 4096
DEPTH = 2

CTX_LEN = 256
GRID_W = 64
BRANCH_W = D_MODEL // 2
N_BRANCH = 3
RWKV_HEAD = 64
RWKV_HEADS = BRANCH_W // RWKV_HEAD
DECAY_LORA = 64
ICLR_LORA = 64
GATE_LORA = 128
RWKV_COLS = 3 * BRANCH_W + 2 * DECAY_LORA + 2 * ICLR_LORA + GATE_LORA
RWKV_LN_EPS = 64e-5
RET_QK = 64
RET_V = 128
RET_HEADS = BRANCH_W // RET_V
RET_CHUNK = 128
RET_COLS = 2 * RET_HEADS * RET_QK + 2 * BRANCH_W
RET_LN_EPS = 1e-5
MLA_NOPE = 64
MLA_ROPE = 32
MLA_V = 64
MLA_HEADS = BRANCH_W // MLA_V
MLA_Q_RANK = 384
MLA_KV_RANK = 256
MLA_COLS = MLA_Q_RANK + MLA_KV_RANK + MLA_ROPE
GATE_COLS = N_BRANCH * D_MODEL
N_IN = RWKV_COLS + RET_COLS + MLA_COLS + GATE_COLS
Q_BLOCK = 128
ROPE_BASE = 10000.0
D_FF = 2816
N_EXPERTS = 8
TOP_K = 2
D_FF_EXPERT = 1408
RMS_EPS = 1e-6
F32 = jnp.float32

kernel_name = 'hybrid_rwkv7_retnet_mla_moe_dit_block'


def rms_norm(x, gain, eps=RMS_EPS):
    xf = x.astype(F32)
    y = xf * lax.rsqrt(jnp.mean(jnp.square(xf), axis=-1, keepdims=True) + eps)
    return (y * gain.astype(F32)).astype(x.dtype)


def modulate(h, gain, shift, scale):
    return rms_norm(h, gain) * (1 + scale) + shift


def head_norm(y, n_heads, gain, bias, eps):
    B, T, C = y.shape
    yf = y.astype(F32).reshape(B, T, n_heads, C // n_heads)
    mu = jnp.mean(yf, axis=-1, keepdims=True)
    var = jnp.mean(jnp.square(yf - mu), axis=-1, keepdims=True)
    yn = ((yf - mu) * lax.rsqrt(var + eps)).reshape(B, T, C)
    return (yn * gain.astype(F32) + bias.astype(F32)).astype(y.dtype)


def split_last(u, sizes):
    return jnp.split(u, [int(s) for s in np.cumsum(sizes)[:-1]], axis=-1)


def centred_shift(u):
    zero = jnp.zeros_like(u[:, :1])
    prev = jnp.concatenate([zero, u[:, :-1]], axis=1)
    nxt = jnp.concatenate([u[:, 1:], zero], axis=1)
    return 0.5 * (prev + nxt)


def axial_rope(n_tokens, rot_dim):
    rows = n_tokens // GRID_W
    row = jnp.repeat(jnp.arange(rows, dtype=F32), GRID_W)
    col = jnp.tile(jnp.arange(GRID_W, dtype=F32), rows)
    n_freq = rot_dim // 4
    inv = jnp.power(ROPE_BASE, -jnp.arange(n_freq, dtype=F32) / n_freq)
    ang = jnp.concatenate([row[:, None] * inv, col[:, None] * inv], axis=-1)
    return jnp.cos(ang), jnp.sin(ang)


def apply_rope(x, cos, sin):
    half = x.shape[-1] // 2
    x1, x2 = x[..., :half], x[..., half:]
    cs = cos[None, :, None, :].astype(x.dtype)
    sn = sin[None, :, None, :].astype(x.dtype)
    return jnp.concatenate([x1 * cs - x2 * sn, x1 * sn + x2 * cs], axis=-1)


def rwkv7_scan(S0, r, w, k, v, kk, a):
    def step(S, xs):
        r_t, w_t, k_t, v_t, kk_t, a_t = xs
        sa = jnp.einsum('bhij,bhj->bhi', S, -kk_t)
        S = (S * w_t[:, :, None, :] + sa[..., None] * (kk_t * a_t)[:, :, None, :]
             + v_t[..., None] * k_t[:, :, None, :])
        return S, jnp.einsum('bhij,bhj->bhi', S, r_t)
    xs = tuple(jnp.moveaxis(t, 1, 0) for t in (r, w, k, v, kk, a))
    S, y = lax.scan(step, S0, xs)
    return jnp.moveaxis(y, 0, 1), S


def rwkv7_mixer(u, mu, w0, w2, a0, a2, gate_up, k_k, k_a, r_k, ln_g, ln_b, init_states, need_out):
    B, T, _ = u.shape
    H, N = RWKV_HEADS, RWKV_HEAD
    u = u + mu * (centred_shift(u) - u)
    r, k, v, wd, ad, gd = split_last(u, [BRANCH_W] * 3 + [2 * DECAY_LORA, 2 * ICLR_LORA, GATE_LORA])

    def heads(t):
        return t.astype(F32).reshape(B, T, H, N)
    r, k, v = heads(r), heads(k), heads(v)
    kk = k * k_k.astype(F32).reshape(H, N)
    kk = kk / jnp.maximum(jnp.linalg.norm(kk, axis=-1, keepdims=True), 1e-12)
    wd = wd.reshape(B, T, 2, DECAY_LORA)
    ad = ad.reshape(B, T, 2, ICLR_LORA)
    scan_sum = 0.0
    k_dirs = []
    finals = []
    for d in range(2):
        z = (w0[d] + jnp.tanh(wd[:, :, d]) @ w2[d]).astype(F32)
        decay = heads(jnp.exp(-jnp.exp(-jax.nn.softplus(-z) - 0.5)))
        a = heads(jax.nn.sigmoid((a0[d] + ad[:, :, d] @ a2[d]).astype(F32)))
        k_d = k * (1.0 + (a - 1.0) * k_a.astype(F32).reshape(H, N))
        seq = (r, decay, k_d, v, kk, a)
        if d == 1:
            seq = tuple(jnp.flip(t, 1) for t in seq)
        o, S = rwkv7_scan(init_states[d], *seq)
        scan_sum = scan_sum + (jnp.flip(o, 1) if d == 1 else o)
        k_dirs.append(k_d)
        finals.append(S)
    if not need_out:
        return None, tuple(finals)
    r_k_h = r_k.astype(F32).reshape(H, N)
    bonus = (jnp.sum(r * k_dirs[0] * r_k_h, -1, keepdims=True)
             + jnp.sum(r * k_dirs[1] * r_k_h, -1, keepdims=True)) * v
    y = (head_norm(scan_sum.reshape(B, T, BRANCH_W), H, ln_g, ln_b, RWKV_LN_EPS)
         + bonus.reshape(B, T, BRANCH_W))
    gate = jax.nn.sigmoid(gd) @ gate_up
    return (y * gate).astype(u.dtype), tuple(finals)


def retention_chunkwise(q, k, v, log_gamma, R0):
    B, T, H, _ = q.shape
    dv = v.shape[-1]
    C = RET_CHUNK
    n = T // C
    pos = jnp.arange(C, dtype=F32)
    diff = pos[:, None] - pos[None, :]
    Dmat = jnp.where(diff >= 0, jnp.exp(jnp.maximum(diff, 0.0)[None] * log_gamma[:, None, None]), 0.0)
    xi = jnp.exp((pos + 1.0)[:, None] * log_gamma[None, :])
    zeta = jnp.exp((C - 1.0 - pos)[:, None] * log_gamma[None, :])
    g_chunk = jnp.exp(C * log_gamma)

    def to_chunks(t):
        return jnp.moveaxis(t.reshape(B, n, C, H, t.shape[-1]), 1, 0)

    def step(R, xs):
        qc, kc, vc = xs
        s = jnp.einsum('bnhd,bmhd->bhnm', qc, kc) * Dmat
        y = jnp.einsum('bhnm,bmhe->bnhe', s, vc)
        y = y + jnp.einsum('bnhd,bhde->bnhe', qc, R) * xi[None, :, :, None]
        R = R * g_chunk[None, :, None, None] + jnp.einsum('bmhd,bmhe->bhde', kc * zeta[None, :, :, None], vc)
        return R, y
    R, y = lax.scan(step, R0, (to_chunks(q), to_chunks(k), to_chunks(v)))
    return jnp.moveaxis(y, 0, 1).reshape(B, T, H, dv), R


def retention_mixer(u, decay_p, ln_g, ln_b, rope, init_states, need_out):
    B, T, _ = u.shape
    q, k, v, g = split_last(u, [RET_HEADS * RET_QK, RET_HEADS * RET_QK, BRANCH_W, BRANCH_W])
    q = q.astype(F32).reshape(B, T, RET_HEADS, RET_QK)
    k = k.astype(F32).reshape(B, T, RET_HEADS, RET_QK) * RET_QK ** -0.5
    v = v.astype(F32).reshape(B, T, RET_HEADS, RET_V)
    if rope is not None:
        q = apply_rope(q, *rope)
        k = apply_rope(k, *rope)
    log_gamma = -jnp.exp(decay_p.astype(F32))
    y_fwd, r_fwd = retention_chunkwise(q, k, v, log_gamma[0], init_states[0])
    y_bwd, r_bwd = retention_chunkwise(jnp.flip(q, 1), jnp.flip(k, 1), jnp.flip(v, 1), log_gamma[1], init_states[1])
    if not need_out:
        return None, (r_fwd, r_bwd)
    y = head_norm((y_fwd + jnp.flip(y_bwd, 1)).reshape(B, T, BRANCH_W), RET_HEADS, ln_g, ln_b, RET_LN_EPS)
    return (jax.nn.silu(g) * y).astype(u.dtype), (r_fwd, r_bwd)


def mla_project(u, q_norm, q_up, kv_norm, kv_up, rope):
    B, T, _ = u.shape
    q_lat, kv_lat, k_rope = split_last(u, [MLA_Q_RANK, MLA_KV_RANK, MLA_ROPE])
    q = (rms_norm(q_lat, q_norm) @ q_up).reshape(B, T, MLA_HEADS, MLA_NOPE + MLA_ROPE)
    kv = (rms_norm(kv_lat, kv_norm) @ kv_up).reshape(B, T, MLA_HEADS, MLA_NOPE + MLA_V)
    q_nope, q_rope = q[..., :MLA_NOPE], q[..., MLA_NOPE:]
    k_nope, v = kv[..., :MLA_NOPE], kv[..., MLA_NOPE:]
    k_rope = k_rope[:, :, None, :]
    if rope is not None:
        q_rope = apply_rope(q_rope, *rope)
        k_rope = apply_rope(k_rope, *rope)
    q = jnp.concatenate([q_nope, q_rope], axis=-1)
    k = jnp.concatenate([k_nope, jnp.broadcast_to(k_rope, (B, T, MLA_HEADS, MLA_ROPE))], axis=-1)
    return q, k, v


def blocked_attention(q, k, v):
    B, T, H, dq = q.shape
    nb = T // Q_BLOCK
    scale = dq ** -0.5
    qb = jnp.moveaxis(q.reshape(B, nb, Q_BLOCK, H, dq), 1, 0)

    def one_block(q_blk):
        s = jnp.einsum('bqhd,bkhd->bhqk', q_blk, k, preferred_element_type=F32) * scale
        p = jax.nn.softmax(s, axis=-1).astype(v.dtype)
        return jnp.einsum('bhqk,bkhd->bqhd', p, v)
    o = lax.map(one_block, qb)
    return jnp.moveaxis(o, 0, 1).reshape(B, T, H * v.shape[-1])


def merge_branches(ys, gate_logits, w_branch, w_out):
    B, T, _ = gate_logits.shape
    up = jnp.einsum('btnc,ncd->btnd', jnp.stack(ys, axis=2), w_branch)
    gates = jax.nn.sigmoid(gate_logits.reshape(B, T, N_BRANCH, D_MODEL))
    return jnp.sum(gates * up, axis=2) @ w_out


def swiglu(x, w_gate, w_up, w_down):
    return (jax.nn.silu(x @ w_gate) * (x @ w_up)) @ w_down


def moe_swiglu(x, router, w_gate, w_up, w_down):
    B, T, D = x.shape
    xf = x.reshape(B * T, D)
    logits = jnp.dot(xf, router, preferred_element_type=F32)
    top_v, top_i = lax.top_k(logits, TOP_K)
    top_w = jax.nn.softmax(top_v, axis=-1)
    comb = jnp.einsum('tk,tke->te', top_w, jax.nn.one_hot(top_i, N_EXPERTS, dtype=F32)).astype(x.dtype)
    out = jnp.zeros_like(xf)
    for e in range(N_EXPERTS):
        out = out + comb[:, e:e + 1] * swiglu(xf, w_gate[e], w_up[e], w_down[e])
    return out.reshape(B, T, D)


def setup_inputs(seed: int = 0) -> dict:
    key = jax.random.key(seed)
    keys = iter(jax.random.split(key, 48))

    def normal(shape, scale):
        return jax.random.normal(next(keys), shape, F32) * scale

    def uniform(shape, lo, hi):
        return jax.random.uniform(next(keys), shape, F32, lo, hi)
    L = DEPTH
    n_dense = (DEPTH + 1) // 2
    n_moe = DEPTH // 2
    base_decay = jnp.log(-jnp.log1p(-jnp.exp2(-5.0 - jnp.arange(RET_HEADS, dtype=F32))))
    return {
        'x': normal((BATCH, SEQ, D_MODEL), 1.0),
        'c': normal((BATCH, D_MODEL), 1.0),
        'ctx': normal((BATCH, CTX_LEN, D_MODEL), 1.0),
        'c_ctx': normal((D_MODEL,), 1.0),
        'norm_mix': 1.0 + normal((L, D_MODEL), 0.02),
        'norm_ffn': 1.0 + normal((L, D_MODEL), 0.02),
        'w_mod': normal((L, D_MODEL, 6 * D_MODEL), 0.5 * D_MODEL ** -0.5),
        'b_mod': normal((L, 6 * D_MODEL), 0.02),
        'w_in': normal((L, D_MODEL, N_IN), D_MODEL ** -0.5),
        'rwkv_mu': uniform((L, RWKV_COLS), 0.2, 0.8),
        'rwkv_w0': uniform((L, 2, BRANCH_W), -6.0, 1.0),
        'rwkv_w2': normal((L, 2, DECAY_LORA, BRANCH_W), 0.5 * DECAY_LORA ** -0.5),
        'rwkv_a0': normal((L, 2, BRANCH_W), 0.1),
        'rwkv_a2': normal((L, 2, ICLR_LORA, BRANCH_W), 0.5 * ICLR_LORA ** -0.5),
        'rwkv_g2': normal((L, GATE_LORA, BRANCH_W), GATE_LORA ** -0.5),
        'rwkv_k_k': 0.85 + normal((L, BRANCH_W), 0.05),
        'rwkv_k_a': 1.0 + normal((L, BRANCH_W), 0.05),
        'rwkv_r_k': normal((L, BRANCH_W), 0.1),
        'rwkv_ln_g': 1.0 + normal((L, BRANCH_W), 0.02),
        'rwkv_ln_b': normal((L, BRANCH_W), 0.02),
        'ret_decay': base_decay + normal((L, 2, RET_HEADS), 0.05),
        'ret_ln_g': 1.0 + normal((L, BRANCH_W), 0.02),
        'ret_ln_b': normal((L, BRANCH_W), 0.02),
        'mla_q_norm': 1.0 + normal((L, MLA_Q_RANK), 0.02),
        'mla_q_up': normal((L, MLA_Q_RANK, MLA_HEADS * (MLA_NOPE + MLA_ROPE)), MLA_Q_RANK ** -0.5),
        'mla_kv_norm': 1.0 + normal((L, MLA_KV_RANK), 0.02),
        'mla_kv_up': normal((L, MLA_KV_RANK, MLA_HEADS * (MLA_NOPE + MLA_V)), MLA_KV_RANK ** -0.5),
        'w_branch': normal((L, N_BRANCH, BRANCH_W, D_MODEL), BRANCH_W ** -0.5),
        'w_out': normal((L, D_MODEL, D_MODEL), D_MODEL ** -0.5),
        'ffn_w_gate': normal((n_dense, D_MODEL, D_FF), D_MODEL ** -0.5),
        'ffn_w_up': normal((n_dense, D_MODEL, D_FF), D_MODEL ** -0.5),
        'ffn_w_down': normal((n_dense, D_FF, D_MODEL), D_FF ** -0.5),
        'moe_router': normal((n_moe, D_MODEL, N_EXPERTS), D_MODEL ** -0.5),
        'moe_w_gate': normal((n_moe, N_EXPERTS, D_MODEL, D_FF_EXPERT), D_MODEL ** -0.5),
        'moe_w_up': normal((n_moe, N_EXPERTS, D_MODEL, D_FF_EXPERT), D_MODEL ** -0.5),
        'moe_w_down': normal((n_moe, N_EXPERTS, D_FF_EXPERT, D_MODEL), D_FF_EXPERT ** -0.5),
        'final_norm': 1.0 + normal((D_MODEL,), 0.02),
    }


def reference(x, c, ctx, c_ctx, norm_mix, norm_ffn, w_mod, b_mod, w_in, rwkv_mu, rwkv_w0, rwkv_w2,
              rwkv_a0, rwkv_a2, rwkv_g2, rwkv_k_k, rwkv_k_a, rwkv_r_k, rwkv_ln_g, rwkv_ln_b, ret_decay,
              ret_ln_g, ret_ln_b, mla_q_norm, mla_q_up, mla_kv_norm, mla_kv_up, w_branch, w_out,
              ffn_w_gate, ffn_w_up, ffn_w_down, moe_router, moe_w_gate, moe_w_up, moe_w_down, final_norm):
    B, n_lat, _ = x.shape
    rope_ret = axial_rope(n_lat, RET_QK)
    rope_mla = axial_rope(n_lat, MLA_ROPE)
    silu_c = jax.nn.silu(c)
    silu_ctx = jax.nn.silu(c_ctx)
    zero_rwkv = jnp.zeros((B, RWKV_HEADS, RWKV_HEAD, RWKV_HEAD), F32)
    zero_ret = jnp.zeros((B, RET_HEADS, RET_QK, RET_V), F32)
    splits = [RWKV_COLS, RET_COLS, MLA_COLS, GATE_COLS]
    h, hc = x, ctx
    for i in range(DEPTH):
        need_ctx = i < DEPTH - 1
        mod = (silu_c @ w_mod[i] + b_mod[i])[:, None, :]
        mod_c = silu_ctx @ w_mod[i] + b_mod[i]
        sh1, sc1, gt1, sh2, sc2, gt2 = jnp.split(mod, 6, axis=-1)
        csh1, csc1, cgt1, csh2, csc2, cgt2 = jnp.split(mod_c, 6, axis=-1)

        ul = modulate(h, norm_mix[i], sh1, sc1) @ w_in[i]
        uc = modulate(hc, norm_mix[i], csh1, csc1) @ w_in[i]
        ul_rwkv, ul_ret, ul_mla, gl_l = split_last(ul, splits)
        uc_rwkv, uc_ret, uc_mla, gl_c = split_last(uc, splits)
        rwkv_p = (rwkv_mu[i], rwkv_w0[i], rwkv_w2[i], rwkv_a0[i], rwkv_a2[i], rwkv_g2[i],
                  rwkv_k_k[i], rwkv_k_a[i], rwkv_r_k[i], rwkv_ln_g[i], rwkv_ln_b[i])
        yc_rwkv, st_rwkv = rwkv7_mixer(uc_rwkv, *rwkv_p, (zero_rwkv, zero_rwkv), need_ctx)
        yl_rwkv, _ = rwkv7_mixer(ul_rwkv, *rwkv_p, st_rwkv, True)
        yc_ret, st_ret = retention_mixer(uc_ret, ret_decay[i], ret_ln_g[i], ret_ln_b[i], None,
                                         (zero_ret, zero_ret), need_ctx)
        yl_ret, _ = retention_mixer(ul_ret, ret_decay[i], ret_ln_g[i], ret_ln_b[i], rope_ret, st_ret, True)
        mla_p = (mla_q_norm[i], mla_q_up[i], mla_kv_norm[i], mla_kv_up[i])
        qc, kc, vc = mla_project(uc_mla, *mla_p, None)
        ql, kl, vl = mla_project(ul_mla, *mla_p, rope_mla)
        yl_mla = blocked_attention(ql, jnp.concatenate([kl, kc], axis=1), jnp.concatenate([vl, vc], axis=1))
        h = h + gt1 * merge_branches((yl_rwkv, yl_ret, yl_mla), gl_l, w_branch[i], w_out[i])
        if need_ctx:
            yc_mla = blocked_attention(qc, kc, vc)
            hc = hc + cgt1 * merge_branches((yc_rwkv, yc_ret, yc_mla), gl_c, w_branch[i], w_out[i])

        j = i // 2
        if i % 2 == 0:
            def channel_mixer(t, j=j):
                return swiglu(t, ffn_w_gate[j], ffn_w_up[j], ffn_w_down[j])
        else:
            def channel_mixer(t, j=j):
                return moe_swiglu(t, moe_router[j], moe_w_gate[j], moe_w_up[j], moe_w_down[j])
        h = h + gt2 * channel_mixer(modulate(h, norm_ffn[i], sh2, sc2))
        if need_ctx:
            hc = hc + cgt2 * channel_mixer(modulate(hc, norm_ffn[i], csh2, csc2))
    return rms_norm(h, final_norm)
```

```python
import numpy as np
import ml_dtypes
import concourse.bass as bass
import concourse.mybir as mybir
from concourse.bass_utils import run_bass_kernel_spmd
from contextlib import ExitStack

F32 = mybir.dt.float32
BF16 = mybir.dt.bfloat16
AF = mybir.ActivationFunctionType
ALU = mybir.AluOpType
AX = mybir.AxisListType
ENGS = ("tensor", "vector", "scalar", "gpsimd", "sync")
V, S, G, PE, SY = "vector", "scalar", "gpsimd", "tensor", "sync"

D = 1024
NCTX = 256
NLAT = 4096
T = NCTX + NLAT
NT = T // 128
U0 = 1920
NCOL = 7808
UROWS = NCOL - U0
DEBUG = {}


class Tl:
    __slots__ = ("ap", "w", "r", "name")

    def __init__(self, ap, name=""):
        self.ap = ap
        self.w = {}
        self.r = {}
        self.name = name

    def __getitem__(self, k):
        return self.ap[k]


class Prog:
    import os
    same_engine_sync = not os.environ.get('NO_SES')

    def __init__(self, nc, n_dma_sems=24):
        self.nc = nc
        self.es = ExitStack()
        self.ops = []
        self.n_dma_sems = n_dma_sems
        self._uid = 0
        self.last = {}
        self.dma_ops = []

    def sb(self, shape, dt=F32, name=None):
        self._uid += 1
        nm = name or f"sb{self._uid}"
        t = self.es.enter_context(self.nc.sbuf_tensor(nm, list(shape), dt))
        return Tl(t, nm)

    def ps(self, shape, dt=F32, name=None):
        self._uid += 1
        nm = name or f"ps{self._uid}"
        t = self.es.enter_context(self.nc.psum_tensor(nm, list(shape), dt))
        return Tl(t, nm)

    def dram(self, name, shape, dt, kind="Internal"):
        t = self.nc.dram_tensor(name, list(shape), dt, kind=kind)
        return Tl(t.ap(), name)

    def op(self, eng, fn, reads=(), writes=(), dma=False):
        idx = len(self.ops)
        deps = set()
        for t in reads:
            deps.update(t.w.values())
        for t in writes:
            deps.update(t.w.values())
            deps.update(t.r.values())
        self.ops.append([eng, fn, deps, dma])
        key = ("dma", idx) if dma else eng
        for t in reads:
            t.r[key] = idx
        for t in writes:
            t.w.clear()
            t.r.clear()
            t.w[key] = idx
        if dma:
            self.dma_ops.append(idx)
        else:
            self.last[eng] = idx
        return idx

    def dma(self, out_t, out_ap, in_t, in_ap, q=SY, **kw):
        return self.op(q, lambda e: e.dma_start(out=out_ap, in_=in_ap, **kw),
                       reads=[in_t], writes=[out_t], dma=True)

    def barrier(self):
        deps = set(self.last.values()) | set(self.dma_ops[-self.n_dma_sems:])
        for e in ENGS:
            self.ops.append([e, (lambda en: en.nop()), set(deps), False])
            self.last[e] = len(self.ops) - 1

    def emit(self):
        nc = self.nc
        ops = self.ops
        n = len(ops)
        has_dep = [False] * n
        for o in ops:
            for d in o[2]:
                has_dep[d] = True
        sem_of = [None] * n
        eng_sem, eng_cnt = {}, {}
        dma_pool, dma_pool_val, dma_prev_op = [], [], []
        with ExitStack() as es:
            LIM = 30000
            counts = {e: 0 for e in ENGS}
            for i, o in enumerate(ops):
                if (not o[3]) and has_dep[i]:
                    counts[o[0]] += 1
            for e in ENGS:
                eng_sem[e] = [es.enter_context(nc.semaphore(f"s_{e}{j}")) for j in range(counts[e] // LIM + 1)]
                eng_cnt[e] = 0
            for i in range(self.n_dma_sems):
                dma_pool.append(es.enter_context(nc.semaphore(f"s_dma{i}")))
                dma_pool_val.append(0)
                dma_prev_op.append(None)
            dma_rr = 0
            extra_wait = {}
            for i, o in enumerate(ops):
                eng, fn, deps, is_dma = o
                if is_dma:
                    s = dma_rr % self.n_dma_sems
                    dma_rr += 1
                    if dma_prev_op[s] is not None:
                        extra_wait[i] = sem_of[dma_prev_op[s]]
                    dma_pool_val[s] += 16
                    sem_of[i] = (dma_pool[s], dma_pool_val[s])
                    dma_prev_op[s] = i
                elif has_dep[i]:
                    sem_of[i] = (eng_sem[eng][eng_cnt[eng] // LIM], eng_cnt[eng] % LIM + 1)
                    eng_cnt[eng] += 1
            per_eng = {e: [] for e in ENGS}
            for i, o in enumerate(ops):
                per_eng[o[0]].append(i)

            def run_engine(ename, eng):
                seen = {}

                def wait(sv):
                    s, v = sv
                    if seen.get(s.name, 0) >= v:
                        return
                    seen[s.name] = v
                    eng.wait_ge(s, v)
                for i in per_eng[ename]:
                    _, fn, deps, is_dma = ops[i]
                    if i in extra_wait:
                        wait(extra_wait[i])
                    for d in sorted(deps):
                        sv = sem_of[d]
                        if sv is None:
                            continue
                        if (not ops[d][3]) and ops[d][0] == ename and (ename == PE or not self.same_engine_sync):
                            continue
                        wait(sv)
                    ins = fn(eng)
                    if sem_of[i] is not None:
                        ins.then_inc(sem_of[i][0], 16 if is_dma else 1)
                if ename == SY:
                    for s in range(self.n_dma_sems):
                        if dma_pool_val[s] > 0:
                            wait((dma_pool[s], dma_pool_val[s]))
                    for e in ENGS:
                        if e != SY and eng_cnt[e] > 0:
                            c = eng_cnt[e] - 1
                            wait((eng_sem[e][c // LIM], c % LIM + 1))

            with nc.Block() as block:
                @block.tensor
                def _(e):
                    run_engine(PE, e)

                @block.vector
                def _(e):
                    run_engine(V, e)

                @block.scalar
                def _(e):
                    run_engine(S, e)

                @block.gpsimd
                def _(e):
                    run_engine(G, e)

                @block.sync
                def _(e):
                    run_engine(SY, e)
        self.es.close()


ARENA = 52300


class KB:
    def __init__(self, nc):
        self.nc = nc
        self.P = Prog(nc)
        self.arena = self.P.sb([128, ARENA], F32, name="arena")
        self.off = 0
        self.rot = 0
        self.dbg = []

    def reset(self):
        self.P.barrier()
        self.off = 0

    def alloc(self, shape, dt=F32, name=""):
        npart = shape[0]
        nel = int(np.prod(shape[1:]))
        words = nel if dt == F32 else (nel + 1) // 2
        words = (words + 15) // 16 * 16
        assert self.off + words <= ARENA, (name, self.off, words)
        ap = self.arena.ap[0:npart, self.off:self.off + words]
        self.off += words
        if dt != F32:
            ap = ap.bitcast(dt)
        ap = ap[:, 0:nel]
        if len(shape) == 3:
            ap = ap.rearrange("p (a b) -> p a b", a=shape[1])
        elif len(shape) == 4:
            ap = ap.rearrange("p (a b c) -> p a b c", a=shape[1], b=shape[2])
        return Tl(ap, name)

    def dma(self, o_t, o, i_t, i, q=SY, **kw):
        return self.P.dma(o_t, o, i_t, i, q=q, **kw)

    def act(self, o_t, o, i_t, i, func, bias=None, scale=None, accum=None, reads=(), writes=()):
        kw = {}
        if bias is not None:
            kw["bias"] = bias
        if scale is not None:
            kw["scale"] = scale
        if accum is not None:
            kw["accum_out"] = accum
        return self.P.op(S, lambda e: e.activation(out=o, in_=i, func=func, **kw),
                         reads=[i_t] + list(reads), writes=[o_t] + list(writes))

    def tt(self, eng, o_t, o, a_t, a, b_t, b, op):
        return self.P.op(eng, lambda e: e.tensor_tensor(out=o, in0=a, in1=b, op=op), reads=[a_t, b_t], writes=[o_t])

    def ts(self, eng, o_t, o, a_t, a, s1, s2, op0, op1=None, reads=(), accum=None, writes=()):
        kw = {}
        if op1 is not None:
            kw["op1"] = op1
        if accum is not None:
            kw["accum_out"] = accum
        return self.P.op(eng, lambda e: e.tensor_scalar(out=o, in0=a, scalar1=s1, scalar2=s2, op0=op0, **kw),
                         reads=[a_t] + list(reads), writes=[o_t] + list(writes))

    def stt(self, o_t, o, a_t, a, sc, b_t, b, op0, op1, reads=(), accum=None, writes=()):
        kw = {}
        if accum is not None:
            kw["accum_out"] = accum
        return self.P.op(V, lambda e: e.scalar_tensor_tensor(out=o, in0=a, scalar=sc, in1=b, op0=op0, op1=op1, **kw),
                         reads=[a_t, b_t] + list(reads), writes=[o_t] + list(writes))

    def cp(self, eng, o_t, o, i_t, i):
        if eng == S:
            return self.P.op(S, lambda e: e.activation(out=o, in_=i, func=AF.Copy), reads=[i_t], writes=[o_t])
        return self.P.op(eng, lambda e: e.tensor_copy(out=o, in_=i), reads=[i_t], writes=[o_t])

    def memset(self, eng, o_t, o, val):
        return self.P.op(eng, lambda e: e.memset(o, val), reads=[], writes=[o_t])

    def mm(self, o_t, o, l_t, l, r_t, r, start=True, stop=True):
        rd = [l_t, r_t] + ([] if start else [o_t])
        return self.P.op(PE, lambda e: e.matmul(o, lhsT=l, rhs=r, start=start, stop=stop), reads=rd, writes=[o_t])

    def tr(self, o_t, o, i_t, i, id_t, ident):
        return self.P.op(PE, lambda e: e.transpose(out=o, in_=i, identity=ident), reads=[i_t, id_t], writes=[o_t])

    def red(self, o_t, o, i_t, i, op=ALU.add, axis=AX.X):
        return self.P.op(V, lambda e: e.tensor_reduce(out=o, in_=i, axis=axis, op=op), reads=[i_t], writes=[o_t])

    def recip(self, o_t, o, i_t, i):
        return self.P.op(V, lambda e: e.reciprocal(out=o, in_=i), reads=[i_t], writes=[o_t])

    def store(self, o_t, o, i_t, i, q=SY):
        return self.P.dma(Tl(o_t.ap, "untracked"), o, i_t, i, q=q)

    def evac_eng(self):
        self.rot += 1
        return V if self.rot % 2 else S

    def dump(self, name, t, ap, shape, dt=F32):
        d = self.P.dram("dbg_" + name, shape, dt, kind="ExternalOutput")
        self.dma(d, d.ap, t, ap, q=SY)
        self.dbg.append("dbg_" + name)


DUMP_TILE = [0, 0]
USE_F32R = False
TT512 = [(0, 256)] + [(256 + 512 * i, 512) for i in range(8)]
RWKV_EPS = 64e-5
RET_EPS = 1e-5
RMS_EPS = 1e-6


def build(n_layers=2, dbg=False, stop_after=None):
    nc = bass.Bass("TRN2", target_bir_lowering=False)
    kb = KB(nc)
    P = kb.P
    SH = {}

    class LazyI(dict):
        def __missing__(self, name):
            shape, dt = SH[name]
            self[name] = P.dram(name, shape, dt, kind="ExternalInput")
            return self[name]
    I = LazyI()

    def inp(name, shape, dt=F32):
        SH[name] = (shape, dt)
        if not dbg:
            I[name]

    inp("xin", [T, D])
    inp("cvec", [128, 16])
    inp("identb", [128, 128], BF16)
    inp("identf", [128, 128])
    inp("onesf", [128, 128])
    inp("tri", [128, 256])
    inp("mask4", [128, 1024], BF16)
    inp("maskL", [128, 256], BF16)
    inp("invmask", [128, 896], BF16)
    inp("retc", [128, 384])
    inp("rett", [128, 4])
    inp("retrow", [128, 256])
    inp("ropeR", [128, 2 * T], BF16)
    inp("ropeM", [96, 2 * T], BF16)
    inp("ropeK", [32, 2 * T], BF16)
    inp("sel8", [8, 1024])
    inp("fnorm", [1, D])
    for l in range(n_layers):
        inp(f"wmod{l}", [D, 6 * D])
        inp(f"bmod{l}", [128, 48])
        inp(f"nmix{l}", [128, 8])
        inp(f"nffn{l}", [128, 8])
        inp(f"win{l}", [D, NCOL])
        inp(f"mu{l}", [1, 1920])
        inp(f"w0{l}", [2, 512])
        inp(f"a0{l}", [2, 512])
        inp(f"w2{l}", [128, 512])
        inp(f"a2{l}", [128, 512])
        inp(f"g2{l}", [128, 512])
        inp(f"rvec{l}", [5, 512])
        inp(f"rdec{l}", [1, 8])
        inp(f"rln{l}", [128, 8])
        inp(f"mnorm{l}", [128, 5])
        inp(f"qup{l}", [384, 768])
        inp(f"qupsw{l}", [384, 768])
        inp(f"kvk{l}", [256, 512])
        inp(f"kvv{l}", [256, 512])
        inp(f"wbr{l}", [3, 512, D])
        inp(f"wout{l}", [D, D])
    inp("wg0", [D, 2816])
    inp("wu0", [D, 2816])
    inp("wd0", [2816, D])
    if n_layers > 1:
        inp("routerT", [8, D])
        inp("mwg", [8, D, 1408])
        inp("mwu", [8, D, 1408])
        inp("mwd", [8, 1408, D])
    out = P.dram("out", [NLAT, D], F32, kind="ExternalOutput")

    def SK(nm):
        return "ExternalOutput" if (dbg and nm in dbg) else "Internal"
    hbuf = [I["xin"]] + [P.dram(f"h{i}", [T, D], F32, kind=SK(f"h{i}")) for i in range(1, 2 * n_layers + 1)]
    uT = P.dram("uT", [UROWS, T], BF16, kind=SK("uT"))
    yT = P.dram("yT", [1536, T], BF16, kind=SK("yT"))
    modd = [P.dram(f"modd{l}", [128, 128], F32, kind=SK(f"modd{l}")) for l in range(n_layers)]
    rkvd = P.dram("rkvd", [T, 1920], F32, kind=SK("rkvd"))
    ofw = P.dram("ofw", [T, 512], F32, kind=SK("ofw"))
    obw = P.dram("obw", [T, 512], F32, kind=SK("obw"))
    qaug = P.dram("qaug", [8, 97, T], BF16, kind=SK("qaug"))
    kaug = P.dram("kaug", [8, 97, T], BF16, kind=SK("kaug"))

    PS = [P.ps([128, 512], F32, name=f"psb{i}") for i in range(6)]
    PT = [P.ps([128, 1024], BF16, name=f"ptb{i}") for i in range(2)]
    identb = P.sb([128, 128], BF16, name="identb_s")
    identf = P.sb([128, 128], F32, name="identf_s")
    onesf = P.sb([128, 128], F32, name="onesf_s")
    sil = P.sb([128, 8, 2], F32, name="sil")
    epsr = P.sb([128, 4], F32, name="epsr")
    kb.dma(identb, identb[:, :], I["identb"], I["identb"].ap)
    kb.dma(identf, identf[:, :], I["identf"], I["identf"].ap)
    kb.dma(onesf, onesf[:, :], I["onesf"], I["onesf"].ap)
    cv = P.sb([128, 16], F32, name="cv")
    kb.dma(cv, cv[:, :], I["cvec"], I["cvec"].ap)
    kb.act(sil, sil[:, :, :], cv, cv[:, :].rearrange("p (k w) -> p k w", w=2), AF.Silu)
    kb.memset(V, epsr, epsr[:, 0:1], RMS_EPS)
    kb.memset(V, epsr, epsr[:, 1:2], RWKV_EPS)
    kb.memset(V, epsr, epsr[:, 2:3], RET_EPS)
    kb.memset(V, epsr, epsr[:, 3:4], 1e-24)
    MOD = []

    def rowb(l, j0, w, n=8):
        return modd[l].ap.rearrange("(j w) p -> w j p", w=2)[w, j0:j0 + n, :].partition_broadcast(128)

    def phase_mod(l):
        kb.reset()
        wmb = [kb.alloc([128, 8, 512], F32, "wm") for _ in range(2)]
        bm = kb.alloc([128, 48], F32, "bm")
        nm = kb.alloc([128, 16], F32, "nm")
        pk = kb.alloc([128, 128], F32, "pk")
        pkT = kb.alloc([128, 128], F32, "pkT")
        kb.dma(bm, bm[:, :], I[f"bmod{l}"], I[f"bmod{l}"].ap)
        kb.dma(nm, nm[:, 0:8], I[f"nmix{l}"], I[f"nmix{l}"].ap)
        kb.dma(nm, nm[:, 8:16], I[f"nffn{l}"], I[f"nffn{l}"].ap)
        psm = PS[0]
        wsrc = I[f"wmod{l}"].ap.rearrange("(k p) n -> p k n", p=128)
        for cb in range(12):
            wm = wmb[cb % 2]
            kb.dma(wm, wm[:, :, :], I[f"wmod{l}"], wsrc[:, :, cb * 512:(cb + 1) * 512])
            for sub in range(4):
                j = cb * 4 + sub
                for k in range(8):
                    kb.mm(psm, psm[:, 2 * j:2 * j + 2], wm, wm[:, k, sub * 128:(sub + 1) * 128], sil, sil[:, k, :], k == 0, k == 7)
        vec = P.sb([128, 4, 8, 2], F32, name=f"vec{l}")
        kb.memset(V, pk, pk[:, :], 0.0)
        pk3 = pk[:, 0:96].rearrange("p (j w) -> p j w", w=2)
        kb.tt(V, pk, pk3, psm, psm[:, 0:96].rearrange("p (j w) -> p j w", w=2), bm,
              bm[:, :].unsqueeze(2).broadcast_to([128, 48, 2]), ALU.add)
        for (ai, scj, shj, nmo) in ((0, 8, 0, 0), (2, 32, 24, 8)):
            kb.ts(V, vec, vec[:, ai, :, :], pk, pk3[:, scj:scj + 8, :], 1.0, None, ALU.add)
            kb.tt(V, vec, vec[:, ai, :, :], vec, vec[:, ai, :, :], nm,
                  nm[:, nmo:nmo + 8].unsqueeze(2).broadcast_to([128, 8, 2]), ALU.mult)
            kb.cp(V, vec, vec[:, ai + 1, :, :], pk, pk3[:, shj:shj + 8, :])
        kb.cp(V, pk, pk[:, 96:112], vec, vec[:, 2, :, :].rearrange("p k w -> p (k w)"))
        pst = PS[1]
        kb.tr(pst, pst[:, 0:128], pk, pk[:, :], identf, identf[:, :])
        kb.cp(V, pkT, pkT[:, :], pst, pst[:, 0:128])
        kb.dma(modd[l], modd[l].ap, pkT, pkT[:, :])
        MOD.append(vec)

    class NormCtx:
        def __init__(self):
            self.ht = [kb.alloc([128, D], F32, "ht") for _ in range(2)]
            self.junk = kb.alloc([128, D], F32, "junk")
            self.hnb = [kb.alloc([128, D], BF16, "hnb") for _ in range(2)]
            self.st = [kb.alloc([128, 4], F32, "st") for _ in range(2)]
            self.n = 0

    def norm_tile(ncx, i, src, vec, ai, dst_t, dst, keep_hn=None):
        w = 1 if i < 2 else 0
        b = ncx.n % 2
        ncx.n += 1
        ht, hnb, st = ncx.ht[b], ncx.hnb[b], ncx.st[b]
        kb.dma(ht, ht[:, :], src, src[i * 128:(i + 1) * 128, :])
        kb.act(ncx.junk, ncx.junk[:, :], ht, ht[:, :], AF.Square, accum=st[:, 0:1], writes=[st])
        kb.act(st, st[:, 1:2], st, st[:, 0:1], AF.Sqrt, scale=1.0 / D, bias=epsr[:, 0:1], reads=[epsr])
        kb.recip(st, st[:, 2:3], st, st[:, 1:2])
        if keep_hn is not None:
            kb.ts(V, keep_hn, keep_hn[:, :], ht, ht[:, :], st[:, 2:3], None, ALU.mult, reads=[st])
        kb.act(hnb, hnb[:, :], ht, ht[:, :], AF.Copy, scale=st[:, 2:3], reads=[st])
        pt = PT[ncx.n % 2]
        pt3 = pt[:, :].rearrange("p (c t) -> p c t", c=8)
        for c in range(8):
            kb.tr(pt, pt3[:, c, :], hnb, hnb[:, c * 128:(c + 1) * 128], identb, identb[:, :])
        for c in range(8):
            if c % 2 == 0:
                kb.ts(V, dst_t, dst[:, c, :], pt, pt3[:, c, :], vec[:, ai, c, w:w + 1], vec[:, ai + 1, c, w:w + 1],
                      ALU.mult, ALU.add, reads=[vec])
            else:
                kb.act(dst_t, dst[:, c, :], pt, pt3[:, c, :], AF.Identity, scale=vec[:, ai, c, w:w + 1],
                       bias=vec[:, ai + 1, c, w:w + 1], reads=[vec])
        return ht, st

    hm_mark = [0]

    def phase_norm_all(l, src, vec, ai):
        hm = [kb.alloc([128, 8, n], BF16, f"hm{tt}") for tt, (t0, n) in enumerate(TT512)]
        hm_mark[0] = kb.off
        ncx = NormCtx()
        for i in range(NT):
            tok = i * 128
            tt = 0 if tok < 256 else 1 + (tok - 256) // 512
            o = tok - TT512[tt][0]
            norm_tile(ncx, i, src, vec, ai, hm[tt], hm[tt][:, :, o:o + 128])
        return hm

    def phase_proj(l, hm):
        wsrc = I[f"win{l}"].ap.rearrange("(k p) n -> p k n", p=128)
        wtb = [kb.alloc([128, 8, 512], BF16, "wt") for _ in range(2)]
        obs = [kb.alloc([128, 512], BF16, "ob") for _ in range(4)]
        nblk = (UROWS + 511) // 512
        import os
        nblk = int(os.environ.get('PROJ_NBLK', nblk))
        cnt = 0
        for cb in range(nblk):
            c0 = U0 + cb * 512
            ncol = min(512, NCOL - c0)
            wt = wtb[cb % 2]
            kb.dma(wt, wt[:, :, 0:ncol], I[f"win{l}"], wsrc[:, :, c0:c0 + ncol], q=G)
            for sub in range((ncol + 127) // 128):
                m = min(128, ncol - sub * 128)
                r0 = c0 - U0 + sub * 128
                for tt, (t0, n) in enumerate(TT512):
                    ps = PS[cnt % 4]
                    ob = obs[cnt % 4]
                    cnt += 1
                    for k in range(8):
                        kb.mm(ps, ps[0:m, 0:n], wt, wt[:, k, sub * 128:sub * 128 + m], hm[tt], hm[tt][:, k, 0:n], k == 0, k == 7)
                    kb.cp(kb.evac_eng(), ob, ob[0:m, 0:n], ps, ps[0:m, 0:n])
                    kb.store(uT, uT[r0:r0 + m, t0:t0 + n], ob, ob[0:m, 0:n], q=SY)

    def phase_rwkv_pre(l, hm):
        wsrc = I[f"win{l}"].ap.rearrange("(k p) n -> p k n", p=128)
        W1 = kb.alloc([128, 8, 1920], BF16, "W1")
        kb.dma(W1, W1[:, :, :], I[f"win{l}"], wsrc[:, :, 0:1920], q=G)
        m1 = kb.alloc([128, 1920], F32, "m1")
        m2 = kb.alloc([128, 1920], F32, "m2")
        kb.dma(m2, m2[:, :], I[f"mu{l}"], I[f"mu{l}"].ap[0, :].partition_broadcast(128))
        kb.ts(V, m1, m1[:, :], m2, m2[:, :], -1.0, 1.0, ALU.mult, ALU.add)
        kb.ts(V, m2, m2[:, :], m2, m2[:, :], 0.5, None, ALU.mult)
        hsb = [kb.alloc([128, 8, 128], BF16, "hs") for _ in range(2)]
        sh2b = [kb.alloc([128, 512], F32, "sh2") for _ in range(2)]
        rkvb = [kb.alloc([128, 1920], F32, "rkv") for _ in range(2)]

        def hm_slice(k_lo, k_hi):
            res = []
            t = k_lo
            while t < k_hi:
                tt = 0 if t < 256 else 1 + (t - 256) // 512
                t0, n = TT512[tt]
                e = min(k_hi, t0 + n)
                res.append((tt, t - t0, e - t))
                t = e
            return res
        cnt = 0
        for i in range(NT):
            hs, rkv = hsb[i % 2], rkvb[i % 2]
            tok = i * 128
            s0, s1 = (0, 256) if i < 2 else (256, T)
            kb.memset(G, hs, hs[:, :, :], 0.0)
            a, b = max(tok - 1, s0), min(tok + 127, s1)
            for (tt, o, n) in hm_slice(a, b):
                dst0 = (TT512[tt][0] + o + 1) - tok
                kb.cp(G, hs, hs[:, :, dst0:dst0 + n], hm[tt], hm[tt][:, :, o:o + n])
            a, b = max(tok + 1, s0), min(tok + 129, s1)
            for (tt, o, n) in hm_slice(a, b):
                dst0 = (TT512[tt][0] + o - 1) - tok
                kb.tt(G, hs, hs[:, :, dst0:dst0 + n], hs, hs[:, :, dst0:dst0 + n], hm[tt], hm[tt][:, :, o:o + n], ALU.add)
            (tt, o, n), = hm_slice(tok, tok + 128)
            for cb in range(4):
                c0 = cb * 512
                ncol = min(512, 1920 - c0)
                ps, ps2 = PS[(cnt % 3) * 2], PS[(cnt % 3) * 2 + 1]
                sh2 = sh2b[cnt % 2]
                cnt += 1
                for k in range(8):
                    kb.mm(ps, ps[:, 0:ncol], hm[tt], hm[tt][:, k, o:o + 128], W1, W1[:, k, c0:c0 + ncol], k == 0, k == 7)
                for k in range(8):
                    kb.mm(ps2, ps2[:, 0:ncol], hs, hs[:, k, :], W1, W1[:, k, c0:c0 + ncol], k == 0, k == 7)
                kb.tt(V, rkv, rkv[:, c0:c0 + ncol], ps, ps[:, 0:ncol], m1, m1[:, c0:c0 + ncol], ALU.mult)
                kb.tt(V, sh2, sh2[:, 0:ncol], ps2, ps2[:, 0:ncol], m2, m2[:, c0:c0 + ncol], ALU.mult)
                kb.tt(G, rkv, rkv[:, c0:c0 + ncol], rkv, rkv[:, c0:c0 + ncol], sh2, sh2[:, 0:ncol], ALU.add)
            kb.store(rkvd, rkvd[tok:tok + 128, :], rkv, rkv[:, :], q=G)

    def phase_rwkv(l):
        kb.reset()
        F32R = mybir.dt.float32r
        lw = kb.alloc([128, 3, 512], BF16, "lw")
        for j, nmn in enumerate(("w2", "a2", "g2")):
            kb.dma(lw, lw[:, j, :], I[f"{nmn}{l}"], I[f"{nmn}{l}"].ap, q=G)
        pv = kb.alloc([128, 7, 512], F32, "pv")
        for d in range(2):
            kb.dma(pv, pv[:, d, :], I[f"w0{l}"], I[f"w0{l}"].ap[d, :].partition_broadcast(128))
            kb.dma(pv, pv[:, 2 + d, :], I[f"a0{l}"], I[f"a0{l}"].ap[d, :].partition_broadcast(128))
        for j in range(3):
            kb.dma(pv, pv[:, 4 + j, :], I[f"rvec{l}"], I[f"rvec{l}"].ap[j, :].partition_broadcast(128))
        cst = kb.alloc([128, 256], F32, "cst")
        cmk = kb.alloc([128, 1280], BF16, "cmk")
        cm2 = kb.alloc([128, 896], BF16, "cm2")
        kb.dma(cst, cst[:, 0:256], I["tri"], I["tri"].ap)
        kb.dma(cmk, cmk[:, 0:1024], I["mask4"], I["mask4"].ap)
        kb.dma(cmk, cmk[:, 1024:1280], I["maskL"], I["maskL"].ap)
        kb.dma(cm2, cm2[:, :], I["invmask"], I["invmask"].ap)
        tri = cst
        order = {0: [0, 1] + list(range(2, NT)), 1: [1, 0] + list(range(NT - 1, 1, -1))}
        v8 = lambda ap: ap.rearrange("p (h n) -> p h n", h=8)

        class Bufs:
            pass
        BD = []
        for d in range(2):
            b = Bufs()
            b.d = d
            b.H32 = kb.alloc([128, 4, 64], F32, "H32")
            b.Hb = kb.alloc([128, 4, 64], BF16, "Hb")
            kb.memset(V, b.H32, b.H32[:, :, :], 0.0)
            kb.memset(V, b.Hb, b.Hb[:, :, :], 0.0)
            b.rkv = kb.alloc([128, 1920], F32, "rkv")
            b.lo = kb.alloc([128, 384], BF16, "lo")
            b.loT = kb.alloc([128, 3, 128], BF16, "loT")
            for nmn in ("kk", "t1", "t2", "t3", "aa", "kd", "lgw"):
                setattr(b, nmn, kb.alloc([128, 512], F32, nmn))
            b.lP = b.aa
            b.Ot = b.t3
            b.sm = kb.alloc([128, 32], F32, "sm")
            b.TM = kb.alloc([128, 4, 512], BF16, "TM")
            b.Bt = [kb.alloc([128, 512], BF16, "Bt") for _ in range(2)]
            b.Kt = [kb.alloc([128, 512], BF16, "Kt") for _ in range(2)]
            b.Vb = [kb.alloc([128, 512], BF16, "Vb") for _ in range(2)]
            b.FM = [kb.alloc([128, 4, 4, 128], BF16, "FM") for _ in range(2)]
            b.GM = [kb.alloc([128, 8, 384], BF16, "GM") for _ in range(2)]
            b.Otb = kb.alloc([128, 512], F32, "Otb")
            b.inv = []
            for g in range(2):
                iv = Bufs()
                for nmn in ("Xf", "Yf"):
                    setattr(iv, nmn, [kb.alloc([128, 4, 128], F32, nmn) for _ in range(2)])
                for nmn in ("Nb", "NTb", "Eb", "ETb", "P1b", "P2b"):
                    setattr(iv, nmn, kb.alloc([128, 4, 128], BF16, nmn))
                b.inv.append(iv)
            b.Wk = kb.alloc([128, 8, 128], BF16, "Wk")
            b.Gs = kb.alloc([128, 512], BF16, "Gs")
            b.Us = kb.alloc([128, 512], BF16, "Us")
            b.PCc = [kb.alloc([128, 4], F32, "PCc") for _ in range(2)]
            BD.append(b)

        def shared(b, i, par=0):
            rkv, t1, t2, sm, kk = b.rkv, b.t1, b.t2, b.sm, b.kk
            kb.dma(rkv, rkv[:, :], rkvd, rkvd[i * 128:(i + 1) * 128, :])
            yield
            kb.tt(G, t1, t1[:, :], rkv, rkv[:, 512:1024], pv, pv[:, 4, :], ALU.mult)
            yield
            kb.tt(G, t2, t2[:, :], t1, t1[:, :], t1, t1[:, :], ALU.mult)
            yield
            kb.red(sm, sm[:, 0:8], t2, v8(t2[:, :]))
            yield
            kb.act(sm, sm[:, 8:16], sm, sm[:, 0:8], AF.Sqrt)
            yield
            kb.ts(V, sm, sm[:, 8:16], sm, sm[:, 8:16], 1e-12, None, ALU.max)
            yield
            kb.recip(sm, sm[:, 16:24], sm, sm[:, 8:16])
            yield
            kb.tt(V, kk, v8(kk[:, :]), t1, v8(t1[:, :]), sm, sm[:, 16:24].unsqueeze(2).broadcast_to([128, 8, 64]), ALU.mult)
            yield
            kb.cp(G, b.Vb[par], b.Vb[par][:, :], rkv, rkv[:, 1024:1536])
            yield
            kb.act(b.lo, b.lo[:, 0:128], rkv, rkv[:, 1536:1664], AF.Tanh)
            yield
            kb.cp(G, b.lo, b.lo[:, 128:256], rkv, rkv[:, 1664:1792])
            yield
            kb.act(b.lo, b.lo[:, 256:384], rkv, rkv[:, 1792:1920], AF.Sigmoid)
            yield
            pt = PT[b.d]
            for j in range(3):
                kb.tr(pt, pt[:, j * 128:(j + 1) * 128], b.lo, b.lo[:, j * 128:(j + 1) * 128], identb, identb[:, :])
            kb.cp(S, b.loT, b.loT[:, :, :], pt, pt[:, 0:384].rearrange("p (j t) -> p j t", j=3))
            yield

        def akd(b, d, pa):
            o64 = d * 64
            kb.mm(pa, pa[:, :], b.loT, b.loT[o64:o64 + 64, 1, :], lw, lw[o64:o64 + 64, 1, :])
            yield
            kb.tt(V, b.t2, b.t2[:, :], pa, pa[:, :], pv, pv[:, 2 + d, :], ALU.add)
            yield
            kb.act(b.aa, b.aa[:, :], b.t2, b.t2[:, :], AF.Sigmoid)
            yield
            kb.ts(G, b.t2, b.t2[:, :], b.aa, b.aa[:, :], 1.0, -1.0, ALU.mult, ALU.add)
            yield
            kb.tt(G, b.t2, b.t2[:, :], b.t2, b.t2[:, :], pv, pv[:, 5, :], ALU.mult)
            yield
            kb.ts(G, b.t2, b.t2[:, :], b.t2, b.t2[:, :], 1.0, 1.0, ALU.mult, ALU.add)
            yield
            kb.tt(G, b.kd, b.kd[:, :], b.t2, b.t2[:, :], b.rkv, b.rkv[:, 512:1024], ALU.mult)
            yield


        def inverse(chains, par):
            identg = identf[:, :].unsqueeze(1).broadcast_to([128, 4, 128])

            def mk(j):
                return cm2[:, j * 128:(j + 1) * 128].unsqueeze(1).broadcast_to([128, 4, 128])
            v4 = lambda ps: ps[:, :].rearrange("p (a t) -> p a t", a=4)

            def mm4(ps, l_t, r_t):
                for hi in range(4):
                    kb.mm(ps, ps[:, hi * 128:(hi + 1) * 128], l_t, l_t[:, hi, :], r_t, r_t[:, hi, :])
            for (iv, bk) in chains:
                kb.tt(G, iv.P1b, iv.P1b[:, :, :], iv.Xf[par], iv.Xf[par][:, :, :], cm2, mk(0), ALU.mult)
                kb.tt(G, iv.P2b, iv.P2b[:, :, :], iv.Yf[par], iv.Yf[par][:, :, :], cm2, mk(0), ALU.mult)
                kb.tt(V, iv.Nb, iv.Nb[:, :, :], iv.P1b, iv.P1b[:, :, :], identf, identg, ALU.add)
                kb.tt(V, iv.NTb, iv.NTb[:, :, :], iv.P2b, iv.P2b[:, :, :], identf, identg, ALU.add)
                yield
            NLEV = 6
            for lev in range(NLEV):
                for (iv, bk) in chains:
                    kb.tt(G, iv.Eb, iv.Eb[:, :, :], iv.Xf[par], iv.Xf[par][:, :, :], cm2, mk(1 + lev), ALU.mult)
                    kb.tt(G, iv.ETb, iv.ETb[:, :, :], iv.Yf[par], iv.Yf[par][:, :, :], cm2, mk(1 + lev), ALU.mult)
                    yield
                for (iv, (pa_, pat_)) in chains:
                    mm4(pa_, iv.ETb, iv.Nb)
                    mm4(pat_, iv.Eb, iv.NTb)
                    kb.cp(S, iv.P1b, iv.P1b[:, :, :], pa_, v4(pa_))
                    kb.cp(S, iv.P2b, iv.P2b[:, :, :], pat_, v4(pat_))
                    yield
                for (iv, (pa_, pat_)) in chains:
                    mm4(pa_, iv.NTb, iv.P1b)
                    mm4(pat_, iv.Nb, iv.P2b)
                    kb.tt(V, iv.Nb, iv.Nb[:, :, :], pa_, v4(pa_), iv.Nb, iv.Nb[:, :, :], ALU.add)
                    kb.tt(V, iv.NTb, iv.NTb[:, :, :], pat_, v4(pat_), iv.NTb, iv.NTb[:, :, :], ALU.add)
                    yield

        def direction(i, d, par):
            b = BD[d]
            rkv, kk, t1, t2, t3, aa, kd, lgw, lP, sm = b.rkv, b.kk, b.t1, b.t2, b.t3, b.aa, b.kd, b.lgw, b.lP, b.sm
            TM, Bt, Kt, Vb, FM, GM, Wk, Gs, Us, Ot, PCc, H32, Hb = b.TM, b.Bt[par], b.Kt[par], b.Vb[par], b.FM[par], b.GM[par], b.Wk, b.Gs, b.Us, b.Ot, b.PCc[par], b.H32, b.Hb
            trid = tri[:, d * 128:(d + 1) * 128]
            m4 = cmk[:, d * 512:(d + 1) * 512]
            mL = cmk[:, 1024 + d * 128:1024 + (d + 1) * 128]
            o64 = d * 64
            pz = pa = PS[4 + d]
            kb.mm(pz, pz[:, :], b.loT, b.loT[o64:o64 + 64, 0, :], lw, lw[o64:o64 + 64, 0, :])
            yield
            kb.tt(V, t1, t1[:, :], pz, pz[:, :], pv, pv[:, d, :], ALU.add)
            yield
            kb.act(t1, t1[:, :], t1, t1[:, :], AF.Sigmoid)
            yield
            kb.ts(G, lgw, lgw[:, :], t1, t1[:, :], -0.6065306597126334, 0.0, ALU.mult, ALU.add)
            yield
            yield from akd(b, d, pa)
            kb.tt(G, t3, t3[:, :], kk, kk[:, :], aa, aa[:, :], ALU.mult)
            pl = pc = PS[4 + d]
            kb.mm(pl, pl[:, :], tri, trid, lgw, lgw[:, :])
            yield
            kb.cp(S, lP, lP[:, :], pl, pl[:, :])
            yield
            kb.mm(pc, pc[:, :], onesf, onesf[:, :], lgw, lgw[:, :])
            yield
            kb.tt(V, t1, t1[:, :], lP, lP[:, :], lgw, lgw[:, :], ALU.subtract)
            yield
            kb.act(t1, t1[:, :], t1, t1[:, :], AF.Exp)
            yield
            kb.stt(TM, TM[:, 0, :], t1, t1[:, :], -1.0, kk, kk[:, :], ALU.mult, ALU.mult)
            yield
            kb.act(t1, t1[:, :], lP, lP[:, :], AF.Exp)
            yield
            kb.tt(V, TM, TM[:, 1, :], t1, t1[:, :], rkv, rkv[:, 0:512], ALU.mult)
            yield
            kb.act(t1, t1[:, :], lP, lP[:, :], AF.Exp, scale=-1.0)
            yield
            kb.tt(V, TM, TM[:, 2, :], t3, t3[:, :], t1, t1[:, :], ALU.mult)
            yield
            kb.tt(G, TM, TM[:, 3, :], kd, kd[:, :], t1, t1[:, :], ALU.mult)
            yield
            kb.tt(V, t2, t2[:, :], pc, pc[:, :], lP, lP[:, :], ALU.subtract)
            yield
            kb.act(t2, t2[:, :], t2, t2[:, :], AF.Exp)
            yield
            kb.tt(V, Bt, Bt[:, :], t3, t3[:, :], t2, t2[:, :], ALU.mult)
            yield
            kb.tt(G, Kt, Kt[:, :], kd, kd[:, :], t2, t2[:, :], ALU.mult)
            yield
            pcc = PS[4 + d]
            for p in range(4):
                kb.mm(pcc, pcc[:, 2 * p:2 * p + 2], lgw, lgw[:, p * 128:(p + 1) * 128], onesf, onesf[:, 0:2])
            kb.act(PCc, PCc[:, :], pcc, pcc[:, 0:8].rearrange("p (a b) -> p a b", b=2)[:, :, 0], AF.Exp)
            yield
            for q in range(4):
                pt = PT[d]
                for p in range(4):
                    kb.tr(pt, pt[:, p * 128:(p + 1) * 128], TM, TM[:, q, p * 128:(p + 1) * 128], identb, identb[:, :])
                kb.cp(kb.evac_eng(), FM, FM[:, :, q, :], pt, pt[:, 0:512].rearrange("p (a t) -> p a t", a=4))
                yield
            for h in range(8):
                g, hi = h // 4, h % 4
                iv = b.inv[g]
                p, o = h // 2, (h % 2) * 64
                pg = PS[h % 2]
                rhsAR = FM[o:o + 64, p, 0:2, :]
                kb.mm(pg, pg[:, 0:256], FM, FM[o:o + 64, p, 2, :], FM, rhsAR)
                kb.mm(pg, pg[:, 256:512], FM, FM[o:o + 64, p, 3, :], FM, rhsAR)
                kb.tt(V, iv.Yf[par], iv.Yf[par][:, hi, :], pg, pg[:, 0:128], cmk, m4[:, 0:128], ALU.mult)
                kb.tt(V, GM, GM[:, h, :], pg, pg[:, 128:512], cmk, m4[:, 128:512], ALU.mult)
                pg2 = PS[2 + h % 2]
                kb.mm(pg2, pg2[:, 0:128], FM, FM[o:o + 64, p, 0, :], FM, FM[o:o + 64, p, 2, :])
                kb.tt(V, iv.Xf[par], iv.Xf[par][:, hi, :], pg2, pg2[:, 0:128], cmk, mL, ALU.mult)
                yield
            return

        def post(i, d, par):
            b = BD[d]
            rkv, kk, t1, t2, t3, aa, kd, lgw, lP, sm = b.rkv, b.kk, b.t1, b.t2, b.t3, b.aa, b.kd, b.lgw, b.lP, b.sm
            TM, Bt, Kt, Vb, FM, GM, Wk, Gs, Us, Ot, PCc, H32, Hb = b.TM, b.Bt[par], b.Kt[par], b.Vb[par], b.FM[par], b.GM[par], b.Wk, b.Gs, b.Us, b.Otb, b.PCc[par], b.H32, b.Hb
            pG, pU, pO, pH = PS[0], PS[1], PS[2], PS[3]
            for h in range(8):
                p, o = h // 2, (h % 2) * 64
                kb.mm(pG, pG[:, h * 64:(h + 1) * 64], FM, FM[o:o + 64, p, 0, :], Hb, Hb[o:o + 64, p, :], True, False)
                kb.mm(pG, pG[:, h * 64:(h + 1) * 64], GM, GM[:, h, 128:256], Vb, Vb[:, h * 64:(h + 1) * 64], False, True)
            kb.cp(S, Gs, Gs[:, :], pG, pG[:, :])
            yield
            for h in range(8):
                kb.mm(pU, pU[:, h * 64:(h + 1) * 64], b.inv[h // 4].NTb, b.inv[h // 4].NTb[:, h % 4, :], Gs, Gs[:, h * 64:(h + 1) * 64])
            kb.cp(S, Us, Us[:, :], pU, pU[:, :])
            yield
            for h in range(8):
                p, o = h // 2, (h % 2) * 64
                sl = slice(h * 64, (h + 1) * 64)
                kb.mm(pO, pO[:, sl], FM, FM[o:o + 64, p, 1, :], Hb, Hb[o:o + 64, p, :], True, False)
                kb.mm(pO, pO[:, sl], GM, GM[:, h, 0:128], Us, Us[:, sl], False, False)
                kb.mm(pO, pO[:, sl], GM, GM[:, h, 256:384], Vb, Vb[:, sl], False, True)
            kb.cp(S, Ot, Ot[:, :], pO, pO[:, :])
            yield
            dst = ofw if d == 0 else obw
            kb.store(dst, dst[i * 128:(i + 1) * 128, :], Ot, Ot[:, :], q=S)
            yield
            for h in range(8):
                p, o = h // 2, (h % 2) * 64
                sl = slice(h * 64, (h + 1) * 64)
                kb.mm(pH, pH[o:o + 64, p * 64:(p + 1) * 64], Bt, Bt[:, sl], Us, Us[:, sl], True, False)
                kb.mm(pH, pH[o:o + 64, p * 64:(p + 1) * 64], Kt, Kt[:, sl], Vb, Vb[:, sl], False, True)
            for p in range(4):
                kb.stt(H32, H32[:, p, :], H32, H32[:, p, :], PCc[:, p:p + 1], pH, pH[:, p * 64:(p + 1) * 64],
                       ALU.mult, ALU.add, reads=[PCc])
            kb.cp(V, Hb, Hb[:, :, :], H32, H32[:, :, :])
            yield

        banks = [(PS[0], PS[1]), (PS[2], PS[3])]

        def run(gens):
            gens = list(gens)
            while gens:
                for gn in list(gens):
                    try:
                        next(gn)
                    except StopIteration:
                        gens.remove(gn)

        def E(n, d):
            yield from shared(BD[d], order[d][n], n % 2)
            yield from direction(order[d][n], d, n % 2)
        run([E(0, 0), E(0, 1)])
        for n in range(NT):
            gens = [inverse([(BD[d].inv[g], banks[g]) for g in range(2) for d in range(2)], n % 2)]
            if n + 1 < NT:
                gens += [E(n + 1, 0), E(n + 1, 1)]
            run(gens)
            run([post(order[0][n], 0, n % 2), post(order[1][n], 1, n % 2)])
        P.barrier()
        b = BD[0]
        rkv, t1, t2, sm, aa, kd = b.rkv, b.t1, b.t2, b.sm, b.aa, b.kd
        bsum = b.PCc[0]
        lnp = BD[1].rkv
        ob2 = BD[1].t1
        Ot = BD[1].t2
        yb = BD[1].Gs
        yo = BD[1].Us
        bs8 = BD[1].sm
        for j in range(2):
            kb.dma(lnp, lnp[:, j * 512:(j + 1) * 512], I[f"rvec{l}"], I[f"rvec{l}"].ap[3 + j, :].partition_broadcast(128))
        for i in range(NT):
            for _ in shared(b, i):
                pass
            for d in range(2):
                for _ in akd(b, d, PS[5]):
                    pass
                kb.tt(V, t2, t2[:, :], kd, kd[:, :], pv, pv[:, 6, :], ALU.mult)
                kb.tt(V, t2, t2[:, :], t2, t2[:, :], rkv, rkv[:, 0:512], ALU.mult)
                kb.red(sm, sm[:, 24:32], t2, v8(t2[:, :]))
                if d == 0:
                    kb.cp(V, bs8, bs8[:, 0:8], sm, sm[:, 24:32])
                else:
                    kb.tt(V, bs8, bs8[:, 0:8], bs8, bs8[:, 0:8], sm, sm[:, 24:32], ALU.add)
            kb.dma(Ot, Ot[:, :], ofw, ofw[i * 128:(i + 1) * 128, :])
            kb.dma(ob2, ob2[:, :], obw, obw[i * 128:(i + 1) * 128, :])
            kb.tt(V, t1, t1[:, :], Ot, Ot[:, :], ob2, ob2[:, :], ALU.add)
            kb.red(sm, sm[:, 0:8], t1, v8(t1[:, :]))
            kb.ts(V, sm, sm[:, 0:8], sm, sm[:, 0:8], 1.0 / 64, None, ALU.mult)
            kb.tt(V, t1, v8(t1[:, :]), t1, v8(t1[:, :]), sm, sm[:, 0:8].unsqueeze(2).broadcast_to([128, 8, 64]), ALU.subtract)
            kb.tt(G, t2, t2[:, :], t1, t1[:, :], t1, t1[:, :], ALU.mult)
            kb.red(sm, sm[:, 8:16], t2, v8(t2[:, :]))
            kb.act(sm, sm[:, 8:16], sm, sm[:, 8:16], AF.Sqrt, scale=1.0 / 64, bias=epsr[:, 1:2], reads=[epsr])
            kb.recip(sm, sm[:, 16:24], sm, sm[:, 8:16])
            kb.tt(V, t1, v8(t1[:, :]), t1, v8(t1[:, :]), sm, sm[:, 16:24].unsqueeze(2).broadcast_to([128, 8, 64]), ALU.mult)
            kb.tt(G, t1, t1[:, :], t1, t1[:, :], lnp, lnp[:, 0:512], ALU.mult)
            kb.tt(G, t1, t1[:, :], t1, t1[:, :], lnp, lnp[:, 512:1024], ALU.add)
            kb.tt(V, t2, v8(t2[:, :]), rkv, v8(rkv[:, 1024:1536]), bs8, bs8[:, 0:8].unsqueeze(2).broadcast_to([128, 8, 64]), ALU.mult)
            kb.tt(V, t1, t1[:, :], t1, t1[:, :], t2, t2[:, :], ALU.add)
            pg = PS[4]
            kb.mm(pg, pg[:, :], b.loT, b.loT[:, 2, :], lw, lw[:, 2, :])
            kb.tt(V, yb, yb[:, :], pg, pg[:, :], t1, t1[:, :], ALU.mult)
            pt = PT[1]
            for c in range(4):
                kb.tr(pt, pt[:, c * 128:(c + 1) * 128], yb, yb[:, c * 128:(c + 1) * 128], identb, identb[:, :])
            kb.cp(S, yo, yo[:, :], pt, pt[:, 0:512])
            kb.store(yT, yT[0:512, i * 128:(i + 1) * 128].rearrange("(c p) t -> p c t", p=128), yo, yo[:, :].rearrange("p (c t) -> p c t", c=4), q=S)

    def phase_ret(l):
        kb.reset()
        rc = kb.alloc([128, 384], F32, "rc")
        kb.dma(rc, rc[:, :], I["retc"], I["retc"].ap)
        rt = kb.alloc([128, 4], F32, "rt")
        kb.dma(rt, rt[:, :], I["rett"], I["rett"].ap)
        rrow = kb.alloc([128, 256], F32, "rrow")
        kb.dma(rrow, rrow[:, :], I["retrow"], I["retrow"].ap)
        dec = kb.alloc([128, 8], F32, "dec")
        kb.dma(dec, dec[:, :], I[f"rdec{l}"], I[f"rdec{l}"].ap[0, :].partition_broadcast(128))
        lg = kb.alloc([128, 8], F32, "lg")
        kb.act(lg, lg[:, :], dec, dec[:, :], AF.Exp)
        kb.ts(V, lg, lg[:, :], lg, lg[:, :], -1.0, None, ALU.mult)
        DM = kb.alloc([128, 8, 128], F32, "DM")
        xi = kb.alloc([128, 8, 128], F32, "xi")
        zt = kb.alloc([128, 8], F32, "zt")
        gck = kb.alloc([128, 8], F32, "gck")
        for d in range(2):
            for h in range(4):
                dh = d * 4 + h
                kb.act(DM, DM[:, dh, :], rc, rc[:, 0:128], AF.Exp, scale=lg[:, dh:dh + 1], reads=[lg])
                kb.tt(V, DM, DM[:, dh, :], DM, DM[:, dh, :], rc, rc[:, 128 + d * 128:256 + d * 128], ALU.mult)
                kb.act(xi, xi[:, dh, :], rrow, rrow[:, d * 128:(d + 1) * 128], AF.Exp, scale=lg[:, dh:dh + 1], reads=[lg])
                kb.act(zt, zt[:, dh:dh + 1], rt, rt[:, 2 + d:3 + d], AF.Exp, scale=lg[:, dh:dh + 1], reads=[lg])
        kb.act(gck, gck[:, :], lg, lg[:, :], AF.Exp, scale=128.0)
        rln = kb.alloc([128, 8], F32, "rln")
        kb.dma(rln, rln[:, :], I[f"rln{l}"], I[f"rln{l}"].ap)
        R32 = kb.alloc([128, 4, 128], F32, "R32")
        Rb = kb.alloc([128, 4, 128], BF16, "Rb")
        kb.memset(V, R32, R32[:, :, :], 0.0)
        kb.memset(V, Rb, Rb[:, :, :], 0.0)
        raw = kb.alloc([128, 12, 128], BF16, "raw")
        rope = kb.alloc([128, 2, 128], BF16, "rope")
        qk = kb.alloc([128, 4, 128], F32, "qk")
        tmp = kb.alloc([128, 4, 128], F32, "tmp")
        qkb = kb.alloc([128, 4, 128], BF16, "qkb")
        qx = kb.alloc([128, 2, 128], BF16, "qx")
        ktm = kb.alloc([128, 4, 64], BF16, "ktm")
        ktr = kb.alloc([128, 4, 64], BF16, "ktr")
        vtm = kb.alloc([128, 512], BF16, "vtm")
        PTt = kb.alloc([128, 4, 128], BF16, "PTt")
        yt = kb.alloc([128, 512], F32, "yt")
        y2 = kb.alloc([128, 512], F32, "y2")
        ysq = kb.alloc([128, 512], F32, "ysq")
        ybf = kb.alloc([128, 512], BF16, "ybf")
        smr = kb.alloc([128, 16], F32, "smr")
        gt = kb.alloc([128, 4, 128], BF16, "gt")
        gs = kb.alloc([128, 4, 128], F32, "gs")
        yo = kb.alloc([128, 4, 128], BF16, "yo")
        order = {0: [0, 1] + list(range(2, NT)), 1: [1, 0] + list(range(NT - 1, 1, -1))}

        def load_tile(i):
            sl = slice(i * 128, (i + 1) * 128)
            kb.dma(raw, raw[:, :, :], uT, uT[0:1536, sl].rearrange("(c p) t -> p c t", p=128))
            kb.dma(rope, rope[:, 0, :], I["ropeR"], I["ropeR"].ap[:, sl])
            kb.dma(rope, rope[:, 1, :], I["ropeR"], I["ropeR"].ap[:, T + i * 128:T + (i + 1) * 128])
            kb.tt(V, qk, qk[:, :, :], raw, raw[:, 0:4, :], rope, rope[:, 0:1, :].broadcast_to([128, 4, 128]), ALU.mult)
            kb.tt(V, tmp, tmp[:, :, :], raw, raw[:, 4:8, :], rope, rope[:, 1:2, :].broadcast_to([128, 4, 128]), ALU.mult)
            kb.tt(V, qk, qk[:, :, :], qk, qk[:, :, :], tmp, tmp[:, :, :], ALU.add)
            kb.cp(V, qkb, qkb[:, 0:2, :], qk, qk[:, 0:2, :])
            kb.ts(V, qkb, qkb[:, 2:4, :], qk, qk[:, 2:4, :], 0.125, None, ALU.mult)
            if RCUT < 2:
                return
            pt = PT[0]
            if os.environ.get("RET_VAR") == "c":
                for c in range(4):
                    kb.tr(pt, pt[:, c * 128:(c + 1) * 128], raw, raw[:, 8 + c, :], identb, identb[:, :])
                return
            for c in range(2):
                kb.tr(pt, pt[:, c * 128:(c + 1) * 128], qkb, qkb[:, 2 + c, :], identb, identb[:, :])
            if os.environ.get("RET_VAR") == "b":
                return
            kb.cp(V, ktr, ktr[:, :, :], pt, pt[:, 0:256].rearrange("p (h n) -> p h n", h=4))
            if os.environ.get("RET_VAR") == "a":
                return
            pt2 = PT[1]
            for c in range(4):
                kb.tr(pt2, pt2[:, c * 128:(c + 1) * 128], raw, raw[:, 8 + c, :], identb, identb[:, :])
            kb.cp(S, vtm, vtm[:, :], pt2, pt2[:, 0:512])

        import os
        RCUT = int(os.environ.get("RET_CUT", 100))

        def step(i, d, first):
            load_tile(i)
            if RCUT < 3:
                return
            ps_s, ps_y, ps_r = PS[0], PS[1], PS[2]
            for h in range(4):
                c, o = h // 2, (h % 2) * 64
                pb = ps_s if h % 2 == 0 else PS[3]
                kb.mm(pb, pb[:, c * 128:(c + 1) * 128], qkb, qkb[o:o + 64, 2 + c, :], qkb, qkb[o:o + 64, c, :])
            for h in range(4):
                c = h // 2
                pb = ps_s if h % 2 == 0 else PS[3]
                kb.tt(V, PTt, PTt[:, h, :], pb, pb[:, c * 128:(c + 1) * 128], DM, DM[:, d * 4 + h, :], ALU.mult)
            if RCUT < 4:
                return
            for h in range(4):
                c, o = h // 2, (h % 2) * 64
                dh = d * 4 + h
                kb.tt(V, qx, qx[o:o + 64, c, :], qkb, qkb[o:o + 64, c, :], xi, xi[o:o + 64, dh, :], ALU.mult)
            for h in range(4):
                c, o = h // 2, (h % 2) * 64
                sl = slice(h * 128, (h + 1) * 128)
                kb.mm(ps_y, ps_y[:, sl], PTt, PTt[:, h, :], vtm, vtm[:, sl], True, False)
                kb.mm(ps_y, ps_y[:, sl], qx, qx[o:o + 64, c, :], Rb, Rb[o:o + 64, d * 2 + c, :], False, True)
            if RCUT < 5:
                return
            for h in range(4):
                dh = d * 4 + h
                kb.ts(V, ktm, ktm[:, h, :], ktr, ktr[:, h, :], zt[:, dh:dh + 1], None, ALU.mult, reads=[zt])
            for h in range(4):
                c, o = h // 2, (h % 2) * 64
                kb.mm(ps_r, ps_r[o:o + 64, c * 128:(c + 1) * 128], ktm, ktm[:, h, :], vtm, vtm[:, h * 128:(h + 1) * 128])
            for h in range(4):
                c, o = h // 2, (h % 2) * 64
                dh = d * 4 + h
                kb.stt(R32, R32[o:o + 64, d * 2 + c, :], R32, R32[o:o + 64, d * 2 + c, :], gck[o:o + 64, dh:dh + 1],
                       ps_r, ps_r[o:o + 64, c * 128:(c + 1) * 128], ALU.mult, ALU.add, reads=[gck])
            kb.cp(V, Rb, Rb[:, d * 2:d * 2 + 2, :], R32, R32[:, d * 2:d * 2 + 2, :])
            if RCUT < 6:
                return
            sl = slice(i * 128, (i + 1) * 128)
            if first:
                kb.cp(S, yt, yt[:, :], ps_y, ps_y[:, :])
                kb.dma(ofw, ofw[sl, :], yt, yt[:, :], q=S)
            else:
                kb.dma(y2, y2[:, :], ofw, ofw[sl, :])
                kb.tt(V, yt, yt[:, :], ps_y, ps_y[:, :], y2, y2[:, :], ALU.add)
                v3 = lambda ap: ap.rearrange("p (h n) -> p h n", h=4)
                kb.red(smr, smr[:, 0:4], yt, v3(yt[:, :]))
                kb.ts(V, smr, smr[:, 0:4], smr, smr[:, 0:4], 1.0 / 128, None, ALU.mult)
                kb.tt(V, yt, v3(yt[:, :]), yt, v3(yt[:, :]), smr, smr[:, 0:4].unsqueeze(2).broadcast_to([128, 4, 128]), ALU.subtract)
                kb.tt(V, ysq, ysq[:, :], yt, yt[:, :], yt, yt[:, :], ALU.mult)
                kb.red(smr, smr[:, 4:8], ysq, v3(ysq[:, :]))
                kb.act(smr, smr[:, 4:8], smr, smr[:, 4:8], AF.Sqrt, scale=1.0 / 128, bias=epsr[:, 2:3], reads=[epsr])
                kb.recip(smr, smr[:, 8:12], smr, smr[:, 4:8])
                kb.tt(V, ybf, v3(ybf[:, :]), yt, v3(yt[:, :]), smr, smr[:, 8:12].unsqueeze(2).broadcast_to([128, 4, 128]), ALU.mult)
                pt = PT[0]
                for c in range(4):
                    kb.tr(pt, pt[:, c * 128:(c + 1) * 128], ybf, ybf[:, c * 128:(c + 1) * 128], identb, identb[:, :])
                kb.dma(gt, gt[:, :, :], uT, uT[1536:2048, sl].rearrange("(c p) t -> p c t", p=128))
                kb.act(gs, gs[:, :, :], gt, gt[:, :, :], AF.Silu)
                for c in range(4):
                    kb.ts(V, y2, y2[:, c * 128:(c + 1) * 128], pt, pt[:, c * 128:(c + 1) * 128], rln[:, c:c + 1], rln[:, 4 + c:5 + c],
                          ALU.mult, ALU.add, reads=[rln])
                kb.tt(V, yo, yo[:, :, :], y2, y2[:, :].rearrange("p (c t) -> p c t", c=4), gs, gs[:, :, :], ALU.mult)
                kb.store(yT, yT[512:1024, sl].rearrange("(c p) t -> p c t", p=128), yo, yo[:, :, :], q=G)

        import os
        nst = int(os.environ.get("RET_STEPS", 100))
        for i in order[0][:nst]:
            step(i, 0, True)
        for i in order[1][:max(0, nst - 34)]:
            step(i, 1, False)

    def phase_mla(l):
        kb.reset()
        SC = 96 ** -0.5
        mn = kb.alloc([128, 5], F32, "mn")
        kb.dma(mn, mn[:, :], I[f"mnorm{l}"], I[f"mnorm{l}"].ap)
        qup = kb.alloc([128, 3, 768], BF16, "qup")
        qsw = kb.alloc([128, 3, 768], BF16, "qsw")
        kvk = kb.alloc([128, 2, 512], BF16, "kvk")
        kvv = kb.alloc([128, 2, 512], BF16, "kvv")
        kb.dma(qup, qup[:, :, :], I[f"qup{l}"], I[f"qup{l}"].ap.rearrange("(c p) n -> p c n", p=128), q=G)
        kb.dma(qsw, qsw[:, :, :], I[f"qupsw{l}"], I[f"qupsw{l}"].ap.rearrange("(c p) n -> p c n", p=128), q=G)
        kb.dma(kvk, kvk[:, :, :], I[f"kvk{l}"], I[f"kvk{l}"].ap.rearrange("(c p) n -> p c n", p=128), q=G)
        kb.dma(kvv, kvv[:, :, :], I[f"kvv{l}"], I[f"kvv{l}"].ap.rearrange("(c p) n -> p c n", p=128), q=G)
        Vaug = kb.alloc([128, NT, 8, 65], BF16, "Vaug")
        kb.memset(V, Vaug, Vaug[:, :, :, :], 1.0)
        lat = kb.alloc([128, 5, 512], BF16, "lat")
        sq = kb.alloc([128, 5, 512], F32, "sq")
        rr = kb.alloc([128, 2, 512], F32, "rr")
        ln = kb.alloc([128, 5, 512], BF16, "ln")
        tA2 = [kb.alloc([96, 512], F32, "tA") for _ in range(2)]
        tB2 = [kb.alloc([96, 512], F32, "tB") for _ in range(2)]
        qh2 = [kb.alloc([96, 512], BF16, "qh") for _ in range(2)]
        rp = kb.alloc([96, 2, 512], BF16, "rp")
        kr = kb.alloc([32, 4, 512], BF16, "kr")
        krf = kb.alloc([32, 2, 512], F32, "krf")
        krb = kb.alloc([32, 512], BF16, "krb")
        kh2 = [kb.alloc([64, 512], BF16, "kh") for _ in range(2)]
        km = kb.alloc([1, 16], F32, "km")
        rowt2 = [kb.alloc([1, 2, 512], F32, "rowt") for _ in range(2)]
        rowb2 = [kb.alloc([1, 512], BF16, "rowb") for _ in range(2)]
        onesb = kb.alloc([1, T], BF16, "onesb")
        kb.memset(V, km, km[:, :], 0.0)
        kb.memset(V, onesb, onesb[:, :], 1.0)
        for h in range(8):
            kb.dma(kaug, kaug[h, 96:97, :], onesb, onesb[:, :])
        qsrc = 2048
        ksrc = 2432
        rsrc = 5760

        def load_norm(tt):
            t0, n = TT512[tt]
            kb.dma(lat, lat[:, :, 0:n], uT, uT[qsrc:qsrc + 640, t0:t0 + n].rearrange("(c p) t -> p c t", p=128))
            kb.tt(V, sq, sq[:, :, 0:n], lat, lat[:, :, 0:n], lat, lat[:, :, 0:n], ALU.mult)
            for j, (c0, c1, dim) in enumerate(((0, 3, 384), (3, 5, 256))):
                ps = PS[j]
                for c in range(c0, c1):
                    kb.mm(ps, ps[:, 0:n], onesf, onesf[:, :], sq, sq[:, c, 0:n], c == c0, c == c1 - 1)
                kb.act(rr, rr[:, j, 0:n], ps, ps[:, 0:n], AF.Sqrt, scale=1.0 / dim, bias=epsr[:, 0:1], reads=[epsr])
                kb.recip(rr, rr[:, j, 0:n], rr, rr[:, j, 0:n])
                for c in range(c0, c1):
                    kb.stt(ln, ln[:, c, 0:n], lat, lat[:, c, 0:n], mn[:, c:c + 1], rr, rr[:, j, 0:n], ALU.mult, ALU.mult, reads=[mn])

        for tt, (t0, n) in enumerate(TT512):
            load_norm(tt)
            for h in range(8):
                ps = PS[2 + h % 2]
                kh, tA, rowt = kh2[h % 2], tA2[h % 2], rowt2[h % 2]
                for c in range(2):
                    kb.mm(ps, ps[0:64, 0:n], kvk, kvk[:, c, h * 64:(h + 1) * 64], ln, ln[:, 3 + c, 0:n], c == 0, c == 1)
                kb.cp(S, kh, kh[:, 0:n], ps, ps[0:64, 0:n])
                kb.store(kaug, kaug[h, 0:64, t0:t0 + n], kh, kh[:, 0:n], q=S)
                kb.tt(V, tA, tA[0:64, 0:n], ps, ps[0:64, 0:n], kh, kh[:, 0:n], ALU.mult)
                pr = PS[4 + h % 2]
                kb.mm(pr, pr[:, 0:n], onesf, onesf[0:64, :], tA, tA[0:64, 0:n])
                kb.red(rowt, rowt[:, 0, 0:1], pr, pr[0:1, 0:n], op=ALU.max)
                kb.tt(V, km, km[:, h:h + 1], km, km[:, h:h + 1], rowt, rowt[:, 0, 0:1], ALU.max)
            kb.dma(kr, kr[0:16, 0, 0:n], uT, uT[rsrc:rsrc + 16, t0:t0 + n])
            kb.dma(kr, kr[16:32, 0, 0:n], uT, uT[rsrc + 16:rsrc + 32, t0:t0 + n])
            kb.dma(kr, kr[0:16, 1, 0:n], uT, uT[rsrc + 16:rsrc + 32, t0:t0 + n])
            kb.dma(kr, kr[16:32, 1, 0:n], uT, uT[rsrc:rsrc + 16, t0:t0 + n])
            kb.dma(kr, kr[:, 2, 0:n], I["ropeK"], I["ropeK"].ap[:, t0:t0 + n])
            kb.dma(kr, kr[:, 3, 0:n], I["ropeK"], I["ropeK"].ap[:, T + t0:T + t0 + n])
            kb.tt(V, krf, krf[:, :, 0:n], kr, kr[:, 0:2, 0:n], kr, kr[:, 2:4, 0:n], ALU.mult)
            kb.tt(V, krf, krf[:, 0, 0:n], krf, krf[:, 0, 0:n], krf, krf[:, 1, 0:n], ALU.add)
            kb.cp(V, krb, krb[:, 0:n], krf, krf[:, 0, 0:n])
            for h in range(8):
                kb.store(kaug, kaug[h, 64:96, t0:t0 + n], krb, krb[:, 0:n], q=G)
            kb.tt(V, krf, krf[:, 1, 0:n], krf, krf[:, 0, 0:n], krf, krf[:, 0, 0:n], ALU.mult)
            pr = PS[4]
            rowt = rowt2[0]
            kb.mm(pr, pr[:, 0:n], onesf, onesf[0:32, :], krf, krf[:, 1, 0:n])
            kb.red(rowt, rowt[:, 0, 0:1], pr, pr[0:1, 0:n], op=ALU.max)
            kb.tt(V, km, km[:, 8:9], km, km[:, 8:9], rowt, rowt[:, 0, 0:1], ALU.max)
            for sub in range(n // 128):
                i = (t0 + sub * 128) // 128
                ps = PS[5]
                for c in range(2):
                    kb.mm(ps, ps[:, :], ln, ln[:, 3 + c, sub * 128:(sub + 1) * 128], kvv, kvv[:, c, :], c == 0, c == 1)
                kb.cp(V, Vaug, Vaug[:, i, :, 0:64], ps, ps[:, :].rearrange("p (h e) -> p h e", h=8))
        kb.ts(V, km, km[:, 0:8], km, km[:, 0:8], km[:, 8:9], None, ALU.add)
        kb.act(km, km[:, 0:8], km, km[:, 0:8], AF.Sqrt)
        kb.ts(V, km, km[:, 0:8], km, km[:, 0:8], -1.0, None, ALU.mult)
        for tt, (t0, n) in enumerate(TT512):
            load_norm(tt)
            kb.dma(rp, rp[:, 0, 0:n], I["ropeM"], I["ropeM"].ap[:, t0:t0 + n])
            kb.dma(rp, rp[:, 1, 0:n], I["ropeM"], I["ropeM"].ap[:, T + t0:T + t0 + n])
            for h in range(8):
                pa, pb = PS[(h % 2) * 2], PS[(h % 2) * 2 + 1]
                tA, tB, qh, rowt, rowb_ = tA2[h % 2], tB2[h % 2], qh2[h % 2], rowt2[h % 2], rowb2[h % 2]
                for c in range(3):
                    kb.mm(pa, pa[0:96, 0:n], qup, qup[:, c, h * 96:(h + 1) * 96], ln, ln[:, c, 0:n], c == 0, c == 2)
                for c in range(3):
                    kb.mm(pb, pb[0:96, 0:n], qsw, qsw[:, c, h * 96:(h + 1) * 96], ln, ln[:, c, 0:n], c == 0, c == 2)
                kb.tt(V, tA, tA[:, 0:n], pa, pa[0:96, 0:n], rp, rp[:, 0, 0:n], ALU.mult)
                kb.tt(V, tB, tB[:, 0:n], pb, pb[0:96, 0:n], rp, rp[:, 1, 0:n], ALU.mult)
                kb.tt(V, tA, tA[:, 0:n], tA, tA[:, 0:n], tB, tB[:, 0:n], ALU.add)
                kb.cp(V, qh, qh[:, 0:n], tA, tA[:, 0:n])
                kb.store(qaug, qaug[h, 0:96, t0:t0 + n], qh, qh[:, 0:n], q=G)
                kb.tt(V, tB, tB[:, 0:n], tA, tA[:, 0:n], tA, tA[:, 0:n], ALU.mult)
                pr = PS[4 + h % 2]
                kb.mm(pr, pr[:, 0:n], onesf, onesf[0:96, :], tB, tB[:, 0:n])
                kb.act(rowt, rowt[:, 1, 0:n], pr, pr[0:1, 0:n], AF.Sqrt)
                kb.ts(V, rowb_, rowb_[:, 0:n], rowt, rowt[:, 1, 0:n], km[:, h:h + 1], None, ALU.mult, reads=[km])
                kb.store(qaug, qaug[h, 96:97, t0:t0 + n], rowb_, rowb_[:, 0:n], q=G)
        P.barrier()
        import os
        if os.environ.get('MLA_SKIP3'):
            return
        ka = kb.alloc([97, T], BF16, "ka")
        qa = [kb.alloc([97, 512], BF16, "qa") for _ in range(2)]
        pT = [kb.alloc([128, 512], BF16, "pT") for _ in range(3)]
        rrow2 = kb.alloc([65, 512], F32, "rrow2")
        bc = kb.alloc([64, 512], F32, "bc")
        yo = kb.alloc([64, 512], BF16, "yo")
        cnt = 0
        qcnt = 0
        pending = []
        for h in range(8):
            kb.dma(ka, ka[:, :], kaug, kaug[h, :, :])
            for tt, (t0, n) in enumerate(TT512):
                q_ = qa[tt % 2]
                kb.dma(q_, q_[:, 0:n], qaug, qaug[h, :, t0:t0 + n])
                nk = 2 if tt == 0 else NT
                po = PS[2 + qcnt % 2]
                qcnt += 1
                prev = None
                for kt in range(nk):
                    ps = PS[kt % 2]
                    pt_ = pT[cnt % 3]
                    cnt += 1
                    kb.mm(ps, ps[:, 0:n], ka, ka[:, kt * 128:(kt + 1) * 128], q_, q_[:, 0:n])
                    if prev is not None:
                        pk, ppt = prev
                        kb.mm(po, po[0:65, 0:n], Vaug, Vaug[:, pk, h, :], ppt, ppt[:, 0:n], pk == 0, False)
                    kb.act(pt_, pt_[:, 0:n], ps, ps[:, 0:n], AF.Exp, scale=SC)
                    prev = (kt, pt_)
                    if kt == 1 and len(pending) >= 1:
                        pending.pop(0)()
                pk, ppt = prev
                kb.mm(po, po[0:65, 0:n], Vaug, Vaug[:, pk, h, :], ppt, ppt[:, 0:n], pk == 0, True)
                def fin(po=po, n=n, h=h, t0=t0):
                    kb.recip(rrow2, rrow2[64:65, 0:n], po, po[64:65, 0:n])
                    pb = PS[4]
                    kb.mm(pb, pb[:, 0:n], onesf, onesf[64:65, :], rrow2, rrow2[64:65, 0:n])
                    kb.cp(S, bc, bc[:, 0:n], pb, pb[0:64, 0:n])
                    kb.tt(V, yo, yo[:, 0:n], po, po[0:64, 0:n], bc, bc[:, 0:n], ALU.mult)
                    kb.store(yT, yT[1024 + h * 64:1024 + (h + 1) * 64, t0:t0 + n], yo, yo[:, 0:n], q=G)
                pending.append(fin)

        while pending:
            pending.pop(0)()

    def phase_merge(l, hsrc, hdst):
        kb.reset()
        wb = kb.alloc([128, 12, D], BF16, "wb")
        wo = kb.alloc([128, 8, D], BF16, "wo")
        for nb in range(3):
            kb.dma(wb, wb[:, nb * 4:(nb + 1) * 4, :], I[f"wbr{l}"], I[f"wbr{l}"].ap[nb].rearrange("(c p) n -> p c n", p=128), q=G)
        kb.dma(wo, wo[:, :, :], I[f"wout{l}"], I[f"wout{l}"].ap.rearrange("(c p) n -> p c n", p=128), q=G)
        gtb = kb.alloc([128, 2, D], F32, "gtb")
        for w in range(2):
            kb.dma(gtb, gtb[:, w, :].rearrange("p (j q) -> p j q", j=8), modd[l], rowb(l, 16, w))
        yt = kb.alloc([128, 12, 512], BF16, "yt")
        gl = kb.alloc([128, 24, 512], BF16, "gl")
        sg = kb.alloc([128, 512], F32, "sg")
        acc = kb.alloc([128, 512], F32, "acc")
        tm = kb.alloc([128, 512], F32, "tm")
        mT = kb.alloc([128, 8, 512], BF16, "mT")
        hres = [kb.alloc([128, D], F32, "hres") for _ in range(2)]
        hn = [kb.alloc([128, D], F32, "hn") for _ in range(2)]
        cnt = 0
        for tt, (t0, n) in enumerate(TT512):
            kb.dma(yt, yt[:, :, 0:n], yT, yT[:, t0:t0 + n].rearrange("(c p) t -> p c t", p=128))
            kb.dma(gl, gl[:, :, 0:n], uT, uT[2688:5760, t0:t0 + n].rearrange("(c p) t -> p c t", p=128))
            for oc in range(8):
                for nb in range(3):
                    ps = PS[nb]
                    for kc in range(4):
                        kb.mm(ps, ps[:, 0:n], wb, wb[:, nb * 4 + kc, oc * 128:(oc + 1) * 128], yt, yt[:, nb * 4 + kc, 0:n], kc == 0, kc == 3)
                    kb.act(sg, sg[:, 0:n], gl, gl[:, nb * 8 + oc, 0:n], AF.Sigmoid)
                    if nb == 0:
                        kb.tt(V, acc, acc[:, 0:n], ps, ps[:, 0:n], sg, sg[:, 0:n], ALU.mult)
                    else:
                        kb.tt(V, tm, tm[:, 0:n], ps, ps[:, 0:n], sg, sg[:, 0:n], ALU.mult)
                        kb.tt(G, acc, acc[:, 0:n], acc, acc[:, 0:n], tm, tm[:, 0:n], ALU.add)
                kb.cp(V, mT, mT[:, oc, 0:n], acc, acc[:, 0:n])
            w = 1 if tt == 0 else 0
            for sub in range(n // 128):
                tok = t0 + sub * 128
                hr, hn_ = hres[cnt % 2], hn[cnt % 2]
                cnt += 1
                kb.dma(hr, hr[:, :], hsrc, hsrc[tok:tok + 128, :])
                for half in range(2):
                    ps = PS[3 + half]
                    for kc in range(8):
                        kb.mm(ps, ps[:, :], mT, mT[:, kc, sub * 128:(sub + 1) * 128], wo, wo[:, kc, half * 512:(half + 1) * 512], kc == 0, kc == 7)
                    kb.tt(V, hn_, hn_[:, half * 512:(half + 1) * 512], ps, ps[:, :], gtb, gtb[:, w, half * 512:(half + 1) * 512], ALU.mult)
                kb.tt(G, hn_, hn_[:, :], hn_, hn_[:, :], hr, hr[:, :], ALU.add)
                kb.store(hdst, hdst[tok:tok + 128, :], hn_, hn_[:, :], q=G)

    def phase_ffn(l, hsrc, hdst, moe):
        kb.reset()
        vec = MOD[l]
        ncx = NormCtx()
        hm2 = kb.alloc([128, 8, 512], BF16, "hm2")
        rows = kb.alloc([128, 3, D], F32, "rows")
        wg = kb.alloc([128, 8, 1408], BF16, "wg")
        wu = kb.alloc([128, 8, 1408], BF16, "wu")
        wd = kb.alloc([128, 11, D], BF16, "wd")
        hT = kb.alloc([128, 11, 512], BF16, "hT")
        sgl = kb.alloc([128, 512], F32, "sgl")
        hu = kb.alloc([128, 512], F32, "hu")
        acc = kb.alloc([128, 4, D], F32, "acc")
        hr = kb.alloc([128, D], F32, "hr")
        if moe:
            rtb = kb.alloc([128, 8, D], F32, "rtb")
            for e in range(8):
                kb.dma(rtb, rtb[:, e, :], I["routerT"], I["routerT"].ap[e, :].partition_broadcast(128))
            hnk = kb.alloc([128, D], F32, "hnk")
            hmd = kb.alloc([128, D], F32, "hmd")
            jk = kb.alloc([128, D], F32, "jk")
            rs = kb.alloc([128, 64], F32, "rs")
            combT = kb.alloc([8, 512], F32, "combT")
            combb = kb.alloc([128, 8, 512], BF16, "combb")
            sel = kb.alloc([8, 1024], F32, "sel")
            kb.dma(sel, sel[:, :], I["sel8"], I["sel8"].ap)
            experts = [(I["mwg"].ap[e], I["mwu"].ap[e], I["mwd"].ap[e], e) for e in range(8)]
            srcs = (I["mwg"], I["mwu"], I["mwd"])
        else:
            experts = [(I["wg0"].ap[:, hf * 1408:(hf + 1) * 1408], I["wu0"].ap[:, hf * 1408:(hf + 1) * 1408],
                        I["wd0"].ap[hf * 1408:(hf + 1) * 1408, :], None) for hf in range(2)]
            srcs = (I["wg0"], I["wu0"], I["wd0"])
        tiles = list(enumerate(TT512))
        if moe:
            tiles = tiles[1:]
        cur_w = None
        wsc = P.dram(f"wsc{l}", [len(experts), 128, 33792], BF16)
        for ti, (tt, (t0, n)) in enumerate(tiles):
            first_tile = ti == 0
            w = 1 if tt == 0 else 0
            if w != cur_w:
                cur_w = w
                for j, j0 in enumerate((48, 24, 40)):
                    kb.dma(rows, rows[:, j, :].rearrange("p (j q) -> p j q", j=8), modd[l], rowb(l, j0, w))
            nsub = n // 128
            for sub in range(nsub):
                i = (t0 + sub * 128) // 128
                norm_tile(ncx, i, hsrc, vec, 2, hm2, hm2[:, :, sub * 128:(sub + 1) * 128], keep_hn=(hnk if moe else None))
                if moe:
                    kb.tt(V, hmd, hmd[:, :], hnk, hnk[:, :], rows, rows[:, 0, :], ALU.mult)
                    kb.tt(V, hmd, hmd[:, :], hmd, hmd[:, :], rows, rows[:, 1, :], ALU.add)
                    for e in range(8):
                        kb.stt(jk, jk[:, :], hmd, hmd[:, :], 1.0, rtb, rtb[:, e, :], ALU.mult, ALU.mult,
                               accum=rs[:, e:e + 1], writes=[rs])
                    kb.P.op(V, lambda en: en.max(out=rs[:, 8:16], in_=rs[:, 0:8]), reads=[rs], writes=[rs])
                    kb.ts(V, rs, rs[:, 16:24], rs, rs[:, 0:8], rs[:, 9:10], None, ALU.is_ge)
                    kb.ts(V, rs, rs[:, 32:33], rs, rs[:, 8:9], -1.0, None, ALU.mult)
                    kb.act(rs, rs[:, 24:32], rs, rs[:, 0:8], AF.Exp, bias=rs[:, 32:33])
                    kb.tt(V, rs, rs[:, 24:32], rs, rs[:, 24:32], rs, rs[:, 16:24], ALU.mult)
                    kb.red(rs, rs[:, 33:34], rs, rs[:, 24:32])
                    kb.recip(rs, rs[:, 34:35], rs, rs[:, 33:34])
                    kb.ts(V, rs, rs[:, 40:48], rs, rs[:, 24:32], rs[:, 34:35], None, ALU.mult)
                    pc = PS[5]
                    kb.tr(pc, pc[0:8, 0:128], rs, rs[:, 40:48], identf, identf[:, :])
                    kb.cp(V, combT, combT[:, sub * 128:(sub + 1) * 128], pc, pc[0:8, 0:128])
            if moe:
                for e in range(8):
                    pcb = PS[4]
                    kb.mm(pcb, pcb[:, 0:n], sel, sel[:, e * 128:(e + 1) * 128], combT, combT[:, 0:n])
                    kb.cp(V, combb, combb[:, e, 0:n], pcb, pcb[:, 0:n])
            for ei, (ag, au, ad, eidx) in enumerate(experts):
                if first_tile:
                    kb.dma(wg, wg[:, :, :], srcs[0], ag.rearrange("(c p) n -> p c n", p=128), q=G)
                    kb.dma(wu, wu[:, :, :], srcs[1], au.rearrange("(c p) n -> p c n", p=128), q=G)
                    kb.dma(wd, wd[:, :, :], srcs[2], ad.rearrange("(c p) n -> p c n", p=128), q=G)
                    kb.dma(wsc, wsc[ei, :, 0:11264], wg, wg[:, :, :].rearrange("p c n -> p (c n)"))
                    kb.dma(wsc, wsc[ei, :, 11264:22528], wu, wu[:, :, :].rearrange("p c n -> p (c n)"))
                    kb.dma(wsc, wsc[ei, :, 22528:33792], wd, wd[:, :, :].rearrange("p c n -> p (c n)"))
                else:
                    kb.dma(wg, wg[:, :, :].rearrange("p c n -> p (c n)"), wsc, wsc[ei, :, 0:11264])
                    kb.dma(wu, wu[:, :, :].rearrange("p c n -> p (c n)"), wsc, wsc[ei, :, 11264:22528])
                    kb.dma(wd, wd[:, :, :].rearrange("p c n -> p (c n)"), wsc, wsc[ei, :, 22528:33792])
                for fc in range(11):
                    pg, pu = PS[(fc % 2) * 2], PS[(fc % 2) * 2 + 1]
                    for k in range(8):
                        kb.mm(pg, pg[:, 0:n], wg, wg[:, k, fc * 128:(fc + 1) * 128], hm2, hm2[:, k, 0:n], k == 0, k == 7)
                    for k in range(8):
                        kb.mm(pu, pu[:, 0:n], wu, wu[:, k, fc * 128:(fc + 1) * 128], hm2, hm2[:, k, 0:n], k == 0, k == 7)
                    kb.act(sgl, sgl[:, 0:n], pg, pg[:, 0:n], AF.Silu)
                    if eidx is None:
                        kb.tt(V, hT, hT[:, fc, 0:n], pu, pu[:, 0:n], sgl, sgl[:, 0:n], ALU.mult)
                    else:
                        kb.tt(V, hu, hu[:, 0:n], pu, pu[:, 0:n], sgl, sgl[:, 0:n], ALU.mult)
                        kb.tt(G, hT, hT[:, fc, 0:n], hu, hu[:, 0:n], combb, combb[:, eidx, 0:n], ALU.mult)
                for sub in range(nsub):
                    for half in range(2):
                        ps = PS[4 + half]
                        for fc in range(11):
                            kb.mm(ps, ps[:, :], hT, hT[:, fc, sub * 128:(sub + 1) * 128], wd, wd[:, fc, half * 512:(half + 1) * 512], fc == 0, fc == 10)
                        a_ = acc[:, sub, half * 512:(half + 1) * 512]
                        if ei == 0:
                            kb.cp(V, acc, a_, ps, ps[:, :])
                        else:
                            kb.tt(V, acc, a_, ps, ps[:, :], acc, a_, ALU.add)
            for sub in range(nsub):
                tok = t0 + sub * 128
                kb.dma(hr, hr[:, :], hsrc, hsrc[tok:tok + 128, :])
                kb.tt(V, acc, acc[:, sub, :], acc, acc[:, sub, :], rows, rows[:, 2, :], ALU.mult)
                kb.tt(G, acc, acc[:, sub, :], acc, acc[:, sub, :], hr, hr[:, :], ALU.add)
                kb.store(hdst, hdst[tok:tok + 128, :], acc, acc[:, sub, :], q=G)

    def phase_final(hsrc):
        kb.reset()
        fn = kb.alloc([128, D], F32, "fn")
        kb.dma(fn, fn[:, :], I["fnorm"], I["fnorm"].ap[0, :].partition_broadcast(128))
        hts = [kb.alloc([128, D], F32, "ht") for _ in range(2)]
        jk = kb.alloc([128, D], F32, "jk")
        sts = [kb.alloc([128, 4], F32, "st") for _ in range(2)]
        for i in range(2, NT):
            ht, st = hts[i % 2], sts[i % 2]
            kb.dma(ht, ht[:, :], hsrc, hsrc[i * 128:(i + 1) * 128, :])
            kb.act(jk, jk[:, :], ht, ht[:, :], AF.Square, accum=st[:, 0:1], writes=[st])
            kb.act(st, st[:, 1:2], st, st[:, 0:1], AF.Sqrt, scale=1.0 / D, bias=epsr[:, 0:1], reads=[epsr])
            kb.recip(st, st[:, 2:3], st, st[:, 1:2])
            kb.stt(ht, ht[:, :], ht, ht[:, :], st[:, 2:3], fn, fn[:, :], ALU.mult, ALU.mult, reads=[st])
            kb.store(out, out[(i - 2) * 128:(i - 1) * 128, :], ht, ht[:, :], q=G)

    want = set(stop_after) if stop_after is not None else None

    def on(name):
        return want is None or name in want
    for l in range(n_layers):
        if on(f"mod{l}"):
            phase_mod(l)
        else:
            MOD.append(P.sb([128, 4, 8, 2], F32, name=f"vec{l}"))
    hi = 0
    for l in range(n_layers):
        if on(f"norm{l}"):
            kb.reset()
            hm = phase_norm_all(l, hbuf[hi], MOD[l], 0)
            P.barrier()
            mark = hm_mark[0]
            kb.off = mark
            if on(f"rwkv{l}"):
                phase_rwkv_pre(l, hm)
                P.barrier()
            kb.off = mark
            if on(f"proj{l}"):
                phase_proj(l, hm)
        if on(f"rwkv{l}"):
            phase_rwkv(l)
        if on(f"ret{l}"):
            phase_ret(l)
        if on(f"mla{l}"):
            phase_mla(l)
        if on(f"merge{l}"):
            phase_merge(l, hbuf[hi], hbuf[hi + 1])
        if on(f"ffn{l}"):
            phase_ffn(l, hbuf[hi + 1], hbuf[hi + 2], moe=(l % 2 == 1))
        hi += 2
    if on("final"):
        phase_final(hbuf[hi])
    P.emit()
    nc._used_inputs = list(I.keys())
    return nc


def _fm(v, n):
    return np.ascontiguousarray(np.asarray(v, np.float32).reshape(n, 128).T)


def _rope_tables(n_tokens, rot_dim):
    rows = n_tokens // 64
    row = np.repeat(np.arange(rows, dtype=np.float32), 64)
    col = np.tile(np.arange(64, dtype=np.float32), rows)
    n_freq = rot_dim // 4
    inv = np.power(np.float32(10000.0), -np.arange(n_freq, dtype=np.float32) / n_freq).astype(np.float32)
    ang = np.concatenate([row[:, None] * inv, col[:, None] * inv], axis=-1)
    return np.cos(ang).astype(np.float32), np.sin(ang).astype(np.float32)


def _consts():
    bf = ml_dtypes.bfloat16
    c = {}
    c["identb"] = np.eye(128, dtype=np.float32).astype(bf)
    c["identf"] = np.eye(128, dtype=np.float32)
    c["onesf"] = np.ones((128, 128), np.float32)
    s = np.arange(128)[:, None]
    t = np.arange(128)[None, :]
    c["tri"] = np.concatenate([(s <= t), (s >= t)], 1).astype(np.float32)
    fs, fi = (t > s), (t >= s)
    bs, bi = (t < s), (t <= s)
    c["mask4"] = np.concatenate([fs, fi, fs, fi, bs, bi, bs, bi], 1).astype(np.float32).astype(bf)
    c["maskL"] = np.concatenate([(s > t), (s < t)], 1).astype(np.float32).astype(bf)
    blk = lambda b: (s // b) == (t // b)
    c["invmask"] = np.concatenate([blk(2)] + [blk(2 * q) & ~blk(q) for q in (2, 4, 8, 16, 32, 64)], 1).astype(np.float32).astype(bf)
    c["retc"] = np.concatenate([np.abs(t - s) + 0 * s, (t >= s), (t <= s)], 1).astype(np.float32)
    tt = np.arange(128, dtype=np.float32)
    c["rett"] = np.stack([tt + 1, 128 - tt, 127 - tt, tt], 1).astype(np.float32)
    c["retrow"] = np.ascontiguousarray(np.broadcast_to(np.concatenate([tt + 1, 128 - tt])[None, :], (128, 256))).astype(np.float32)
    cs, sn = _rope_tables(NLAT, 64)
    cosR = np.ones((64, T), np.float32)
    sinR = np.zeros((64, T), np.float32)
    cosR[0:32, NCTX:] = cs.T
    cosR[32:64, NCTX:] = cs.T
    sinR[0:32, NCTX:] = -sn.T
    sinR[32:64, NCTX:] = sn.T
    c["ropeR"] = np.concatenate([np.tile(cosR, (2, 1)), np.tile(sinR, (2, 1))], 1).astype(bf)
    cs, sn = _rope_tables(NLAT, 32)
    cosK = np.ones((32, T), np.float32)
    sinK = np.zeros((32, T), np.float32)
    cosK[0:16, NCTX:] = cs.T
    cosK[16:32, NCTX:] = cs.T
    sinK[0:16, NCTX:] = -sn.T
    sinK[16:32, NCTX:] = sn.T
    c["ropeK"] = np.concatenate([cosK, sinK], 1).astype(bf)
    cosE = np.ones((96, T), np.float32)
    sinE = np.zeros((96, T), np.float32)
    cosE[64:96] = cosK
    sinE[64:96] = sinK
    c["ropeM"] = np.concatenate([cosE, sinE], 1).astype(bf)
    sel = np.zeros((8, 8, 128), np.float32)
    for e in range(8):
        sel[e, e, :] = 1.0
    c["sel8"] = sel.reshape(8, 1024)
    return c


def _swap_halves(w, head, half):
    k, n = w.shape
    w3 = w.reshape(k, n // head, 2, half)
    return np.ascontiguousarray(w3[:, :, ::-1, :].reshape(k, n))


def prep_inputs(inp, b, n_layers=2):
    f = lambda a: np.ascontiguousarray(np.asarray(a, np.float32))
    m = dict(_consts())
    m["xin"] = np.concatenate([f(inp["ctx"][b]), f(inp["x"][b])], 0)
    cv = np.stack([_fm(inp["c"][b], 8), _fm(inp["c_ctx"], 8)], -1)
    m["cvec"] = np.ascontiguousarray(cv.reshape(128, 16))
    m["fnorm"] = f(inp["final_norm"]).reshape(1, D)
    for l in range(n_layers):
        m[f"wmod{l}"] = f(inp["w_mod"][l])
        m[f"bmod{l}"] = _fm(inp["b_mod"][l], 48)
        m[f"nmix{l}"] = _fm(inp["norm_mix"][l], 8)
        m[f"nffn{l}"] = _fm(inp["norm_ffn"][l], 8)
        w = f(inp["w_in"][l])
        rw, rt, ml, gt = w[:, 0:1920], w[:, 1920:3456], w[:, 3456:4128], w[:, 4128:7200]
        rq, rk, rv, rg = rt[:, 0:256], rt[:, 256:512], rt[:, 512:1024], rt[:, 1024:1536]
        m[f"win{l}"] = np.ascontiguousarray(np.concatenate(
            [rw, rq, rk, _swap_halves(rq, 64, 32), _swap_halves(rk, 64, 32), rv, rg, ml[:, 0:384], ml[:, 384:640], gt, ml[:, 640:672], np.zeros((D, 96), np.float32)], 1))
        assert m[f"win{l}"].shape[1] == NCOL
        m[f"mu{l}"] = f(inp["rwkv_mu"][l]).reshape(1, 1920)
        m[f"w0{l}"] = f(inp["rwkv_w0"][l])
        m[f"a0{l}"] = f(inp["rwkv_a0"][l])
        m[f"w2{l}"] = f(inp["rwkv_w2"][l]).reshape(128, 512)
        m[f"a2{l}"] = f(inp["rwkv_a2"][l]).reshape(128, 512)
        m[f"g2{l}"] = f(inp["rwkv_g2"][l])
        m[f"rvec{l}"] = np.stack([f(inp[k][l]) for k in ("rwkv_k_k", "rwkv_k_a", "rwkv_r_k", "rwkv_ln_g", "rwkv_ln_b")], 0)
        m[f"rdec{l}"] = f(inp["ret_decay"][l]).reshape(1, 8)
        m[f"rln{l}"] = np.concatenate([_fm(inp["ret_ln_g"][l], 4), _fm(inp["ret_ln_b"][l], 4)], 1)
        m[f"mnorm{l}"] = np.concatenate([_fm(inp["mla_q_norm"][l], 3), _fm(inp["mla_kv_norm"][l], 2)], 1)
        qu = f(inp["mla_q_up"][l])
        m[f"qup{l}"] = qu
        q3 = qu.reshape(384, 8, 96).copy()
        sw = np.zeros_like(q3)
        sw[:, :, 64:80] = q3[:, :, 80:96]
        sw[:, :, 80:96] = q3[:, :, 64:80]
        m[f"qupsw{l}"] = np.ascontiguousarray(sw.reshape(384, 768))
        kv = f(inp["mla_kv_up"][l]).reshape(256, 8, 128)
        m[f"kvk{l}"] = np.ascontiguousarray(kv[:, :, 0:64].reshape(256, 512))
        m[f"kvv{l}"] = np.ascontiguousarray(kv[:, :, 64:128].reshape(256, 512))
        m[f"wbr{l}"] = f(inp["w_branch"][l])
        m[f"wout{l}"] = f(inp["w_out"][l])
    m["wg0"] = f(inp["ffn_w_gate"][0])
    m["wu0"] = f(inp["ffn_w_up"][0])
    m["wd0"] = f(inp["ffn_w_down"][0])
    if n_layers > 1:
        m["routerT"] = np.ascontiguousarray(f(inp["moe_router"][0]).T)
        m["mwg"] = f(inp["moe_w_gate"][0])
        m["mwu"] = f(inp["moe_w_up"][0])
        m["mwd"] = f(inp["moe_w_down"][0])
    return m


_NC_CACHE = {}


def kernel(**inputs):
    if "nc" not in _NC_CACHE:
        _NC_CACHE["nc"] = build()
    nc = _NC_CACHE["nc"]
    in_maps = [prep_inputs(inputs, b) for b in range(8)]
    res = run_bass_kernel_spmd(nc, in_maps, core_ids=list(range(8)))
    return np.stack([np.asarray(r["out"], np.float32) for r in res.results], 0)
```

```python
import numpy as np
import ml_dtypes
import concourse.bass as bass
import concourse.mybir as mybir
from concourse.bass_utils import run_bass_kernel_spmd
from contextlib import ExitStack

F32 = mybir.dt.float32
BF16 = mybir.dt.bfloat16
AF = mybir.ActivationFunctionType
ALU = mybir.AluOpType
AX = mybir.AxisListType
ENGS = ("tensor", "vector", "scalar", "gpsimd", "sync")
V, S, G, PE, SY = "vector", "scalar", "gpsimd", "tensor", "sync"

D = 1024
NCTX = 256
NLAT = 4096
T = NCTX + NLAT
NT = T // 128
U0 = 1920
NCOL = 7808
UROWS = NCOL - U0
DEBUG = {}


class Tl:
    __slots__ = ("ap", "w", "r", "name")

    def __init__(self, ap, name=""):
        self.ap = ap
        self.w = {}
        self.r = {}
        self.name = name

    def __getitem__(self, k):
        return self.ap[k]


class Prog:
    import os
    same_engine_sync = not os.environ.get('NO_SES')

    def __init__(self, nc, n_dma_sems=24):
        self.nc = nc
        self.es = ExitStack()
        self.ops = []
        self.n_dma_sems = n_dma_sems
        self._uid = 0
        self.last = {}
        self.dma_ops = []

    def sb(self, shape, dt=F32, name=None):
        self._uid += 1
        nm = name or f"sb{self._uid}"
        t = self.es.enter_context(self.nc.sbuf_tensor(nm, list(shape), dt))
        return Tl(t, nm)

    def ps(self, shape, dt=F32, name=None):
        self._uid += 1
        nm = name or f"ps{self._uid}"
        t = self.es.enter_context(self.nc.psum_tensor(nm, list(shape), dt))
        return Tl(t, nm)

    def dram(self, name, shape, dt, kind="Internal"):
        t = self.nc.dram_tensor(name, list(shape), dt, kind=kind)
        return Tl(t.ap(), name)

    def op(self, eng, fn, reads=(), writes=(), dma=False):
        idx = len(self.ops)
        deps = set()
        for t in reads:
            deps.update(t.w.values())
        for t in writes:
            deps.update(t.w.values())
            deps.update(t.r.values())
        self.ops.append([eng, fn, deps, dma])
        key = ("dma", idx) if dma else eng
        for t in reads:
            t.r[key] = idx
        for t in writes:
            t.w.clear()
            t.r.clear()
            t.w[key] = idx
        if dma:
            self.dma_ops.append(idx)
        else:
            self.last[eng] = idx
        return idx

    def dma(self, out_t, out_ap, in_t, in_ap, q=SY, **kw):
        return self.op(q, lambda e: e.dma_start(out=out_ap, in_=in_ap, **kw),
                       reads=[in_t], writes=[out_t], dma=True)

    def barrier(self):
        deps = set(self.last.values()) | set(self.dma_ops[-self.n_dma_sems:])
        for e in ENGS:
            self.ops.append([e, (lambda en: en.nop()), set(deps), False])
            self.last[e] = len(self.ops) - 1

    def emit(self):
        nc = self.nc
        ops = self.ops
        n = len(ops)
        has_dep = [False] * n
        for o in ops:
            for d in o[2]:
                has_dep[d] = True
        sem_of = [None] * n
        eng_sem, eng_cnt = {}, {}
        dma_pool, dma_pool_val, dma_prev_op = [], [], []
        with ExitStack() as es:
            LIM = 30000
            counts = {e: 0 for e in ENGS}
            for i, o in enumerate(ops):
                if (not o[3]) and has_dep[i]:
                    counts[o[0]] += 1
            for e in ENGS:
                eng_sem[e] = [es.enter_context(nc.semaphore(f"s_{e}{j}")) for j in range(counts[e] // LIM + 1)]
                eng_cnt[e] = 0
            for i in range(self.n_dma_sems):
                dma_pool.append(es.enter_context(nc.semaphore(f"s_dma{i}")))
                dma_pool_val.append(0)
                dma_prev_op.append(None)
            dma_rr = 0
            extra_wait = {}
            for i, o in enumerate(ops):
                eng, fn, deps, is_dma = o
                if is_dma:
                    s = dma_rr % self.n_dma_sems
                    dma_rr += 1
                    if dma_prev_op[s] is not None:
                        extra_wait[i] = sem_of[dma_prev_op[s]]
                    dma_pool_val[s] += 16
                    sem_of[i] = (dma_pool[s], dma_pool_val[s])
                    dma_prev_op[s] = i
                elif has_dep[i]:
                    sem_of[i] = (eng_sem[eng][eng_cnt[eng] // LIM], eng_cnt[eng] % LIM + 1)
                    eng_cnt[eng] += 1
            per_eng = {e: [] for e in ENGS}
            for i, o in enumerate(ops):
                per_eng[o[0]].append(i)

            def run_engine(ename, eng):
                seen = {}

                def wait(sv):
                    s, v = sv
                    if seen.get(s.name, 0) >= v:
                        return
                    seen[s.name] = v
                    eng.wait_ge(s, v)
                for i in per_eng[ename]:
                    _, fn, deps, is_dma = ops[i]
                    if i in extra_wait:
                        wait(extra_wait[i])
                    for d in sorted(deps):
                        sv = sem_of[d]
                        if sv is None:
                            continue
                        if (not ops[d][3]) and ops[d][0] == ename and (ename == PE or not self.same_engine_sync):
                            continue
                        wait(sv)
                    ins = fn(eng)
                    if sem_of[i] is not None:
                        ins.then_inc(sem_of[i][0], 16 if is_dma else 1)
                if ename == SY:
                    for s in range(self.n_dma_sems):
                        if dma_pool_val[s] > 0:
                            wait((dma_pool[s], dma_pool_val[s]))
                    for e in ENGS:
                        if e != SY and eng_cnt[e] > 0:
                            c = eng_cnt[e] - 1
                            wait((eng_sem[e][c // LIM], c % LIM + 1))

            with nc.Block() as block:
                @block.tensor
                def _(e):
                    run_engine(PE, e)

                @block.vector
                def _(e):
                    run_engine(V, e)

                @block.scalar
                def _(e):
                    run_engine(S, e)

                @block.gpsimd
                def _(e):
                    run_engine(G, e)

                @block.sync
                def _(e):
                    run_engine(SY, e)
        self.es.close()


ARENA = 52300


class KB:
    def __init__(self, nc):
        self.nc = nc
        self.P = Prog(nc)
        self.arena = self.P.sb([128, ARENA], F32, name="arena")
        self.off = 0
        self.rot = 0
        self.dbg = []

    def reset(self):
        self.P.barrier()
        self.off = 0

    def alloc(self, shape, dt=F32, name=""):
        npart = shape[0]
        nel = int(np.prod(shape[1:]))
        words = nel if dt == F32 else (nel + 1) // 2
        words = (words + 15) // 16 * 16
        assert self.off + words <= ARENA, (name, self.off, words)
        ap = self.arena.ap[0:npart, self.off:self.off + words]
        self.off += words
        if dt != F32:
            ap = ap.bitcast(dt)
        ap = ap[:, 0:nel]
        if len(shape) == 3:
            ap = ap.rearrange("p (a b) -> p a b", a=shape[1])
        elif len(shape) == 4:
            ap = ap.rearrange("p (a b c) -> p a b c", a=shape[1], b=shape[2])
        return Tl(ap, name)

    def dma(self, o_t, o, i_t, i, q=SY, **kw):
        return self.P.dma(o_t, o, i_t, i, q=q, **kw)

    def act(self, o_t, o, i_t, i, func, bias=None, scale=None, accum=None, reads=(), writes=()):
        kw = {}
        if bias is not None:
            kw["bias"] = bias
        if scale is not None:
            kw["scale"] = scale
        if accum is not None:
            kw["accum_out"] = accum
        return self.P.op(S, lambda e: e.activation(out=o, in_=i, func=func, **kw),
                         reads=[i_t] + list(reads), writes=[o_t] + list(writes))

    def tt(self, eng, o_t, o, a_t, a, b_t, b, op):
        return self.P.op(eng, lambda e: e.tensor_tensor(out=o, in0=a, in1=b, op=op), reads=[a_t, b_t], writes=[o_t])

    def ts(self, eng, o_t, o, a_t, a, s1, s2, op0, op1=None, reads=(), accum=None, writes=()):
        kw = {}
        if op1 is not None:
            kw["op1"] = op1
        if accum is not None:
            kw["accum_out"] = accum
        return self.P.op(eng, lambda e: e.tensor_scalar(out=o, in0=a, scalar1=s1, scalar2=s2, op0=op0, **kw),
                         reads=[a_t] + list(reads), writes=[o_t] + list(writes))

    def stt(self, o_t, o, a_t, a, sc, b_t, b, op0, op1, reads=(), accum=None, writes=()):
        kw = {}
        if accum is not None:
            kw["accum_out"] = accum
        return self.P.op(V, lambda e: e.scalar_tensor_tensor(out=o, in0=a, scalar=sc, in1=b, op0=op0, op1=op1, **kw),
                         reads=[a_t, b_t] + list(reads), writes=[o_t] + list(writes))

    def cp(self, eng, o_t, o, i_t, i):
        if eng == S:
            return self.P.op(S, lambda e: e.activation(out=o, in_=i, func=AF.Copy), reads=[i_t], writes=[o_t])
        return self.P.op(eng, lambda e: e.tensor_copy(out=o, in_=i), reads=[i_t], writes=[o_t])

    def memset(self, eng, o_t, o, val):
        return self.P.op(eng, lambda e: e.memset(o, val), reads=[], writes=[o_t])

    def mm(self, o_t, o, l_t, l, r_t, r, start=True, stop=True):
        rd = [l_t, r_t] + ([] if start else [o_t])
        return self.P.op(PE, lambda e: e.matmul(o, lhsT=l, rhs=r, start=start, stop=stop), reads=rd, writes=[o_t])

    def tr(self, o_t, o, i_t, i, id_t, ident):
        return self.P.op(PE, lambda e: e.transpose(out=o, in_=i, identity=ident), reads=[i_t, id_t], writes=[o_t])

    def red(self, o_t, o, i_t, i, op=ALU.add, axis=AX.X):
        return self.P.op(V, lambda e: e.tensor_reduce(out=o, in_=i, axis=axis, op=op), reads=[i_t], writes=[o_t])

    def recip(self, o_t, o, i_t, i):
        return self.P.op(V, lambda e: e.reciprocal(out=o, in_=i), reads=[i_t], writes=[o_t])

    def store(self, o_t, o, i_t, i, q=SY):
        return self.P.dma(Tl(o_t.ap, "untracked"), o, i_t, i, q=q)

    def evac_eng(self):
        self.rot += 1
        return V if self.rot % 2 else S

    def dump(self, name, t, ap, shape, dt=F32):
        d = self.P.dram("dbg_" + name, shape, dt, kind="ExternalOutput")
        self.dma(d, d.ap, t, ap, q=SY)
        self.dbg.append("dbg_" + name)


DUMP_TILE = [0, 0]
import os as _os
RUNW = int(_os.environ.get('RUNW', 3))
USE_F32R = False
TT512 = [(0, 256)] + [(256 + 512 * i, 512) for i in range(8)]
RWKV_EPS = 64e-5
RET_EPS = 1e-5
RMS_EPS = 1e-6


def build(n_layers=2, dbg=False, stop_after=None):
    nc = bass.Bass("TRN2", target_bir_lowering=False)
    kb = KB(nc)
    P = kb.P
    SH = {}

    class LazyI(dict):
        def __missing__(self, name):
            shape, dt = SH[name]
            self[name] = P.dram(name, shape, dt, kind="ExternalInput")
            return self[name]
    I = LazyI()

    def inp(name, shape, dt=F32):
        SH[name] = (shape, dt)
        if not dbg:
            I[name]

    inp("xin", [T, D])
    inp("cvec", [128, 16])
    inp("identb", [128, 128], BF16)
    inp("identf", [128, 128])
    inp("onesf", [128, 128])
    inp("tri", [128, 256])
    inp("mask4", [128, 1024], BF16)
    inp("maskL", [128, 256], BF16)
    inp("invmask", [128, 896], BF16)
    inp("retc", [128, 384])
    inp("rett", [128, 4])
    inp("retrow", [128, 256])
    inp("ropeR", [128, 2 * T], BF16)
    inp("ropeM", [96, 2 * T], BF16)
    inp("ropeK", [32, 2 * T], BF16)
    inp("sel8", [8, 1024])
    inp("fnorm", [1, D])
    for l in range(n_layers):
        inp(f"wmod{l}", [D, 6 * D])
        inp(f"bmod{l}", [128, 48])
        inp(f"nmix{l}", [128, 8])
        inp(f"nffn{l}", [128, 8])
        inp(f"win{l}", [D, NCOL])
        inp(f"mu{l}", [1, 1920])
        inp(f"w0{l}", [2, 512])
        inp(f"a0{l}", [2, 512])
        inp(f"w2{l}", [128, 512])
        inp(f"a2{l}", [128, 512])
        inp(f"g2{l}", [128, 512])
        inp(f"rvec{l}", [5, 512])
        inp(f"rdec{l}", [1, 8])
        inp(f"rln{l}", [128, 8])
        inp(f"mnorm{l}", [128, 5])
        inp(f"qup{l}", [384, 768])
        inp(f"qupsw{l}", [384, 768])
        inp(f"kvk{l}", [256, 512])
        inp(f"kvv{l}", [256, 512])
        inp(f"wbr{l}", [3, 512, D])
        inp(f"wout{l}", [D, D])
    inp("wg0", [D, 2816])
    inp("wu0", [D, 2816])
    inp("wd0", [2816, D])
    if n_layers > 1:
        inp("routerT", [8, D])
        inp("mwg", [8, D, 1408])
        inp("mwu", [8, D, 1408])
        inp("mwd", [8, 1408, D])
    out = P.dram("out", [NLAT, D], F32, kind="ExternalOutput")

    def SK(nm):
        return "ExternalOutput" if (dbg and nm in dbg) else "Internal"
    hbuf = [I["xin"]] + [P.dram(f"h{i}", [T, D], F32, kind=SK(f"h{i}")) for i in range(1, 2 * n_layers + 1)]
    uT = P.dram("uT", [UROWS, T], BF16, kind=SK("uT"))
    yT = P.dram("yT", [1536, T], BF16, kind=SK("yT"))
    modd = [P.dram(f"modd{l}", [128, 128], F32, kind=SK(f"modd{l}")) for l in range(n_layers)]
    rkvd = P.dram("rkvd", [T, 1920], F32, kind=SK("rkvd"))
    ofw = P.dram("ofw", [T, 512], F32, kind=SK("ofw"))
    obw = P.dram("obw", [T, 512], F32, kind=SK("obw"))
    qaug = P.dram("qaug", [8, 97, T], BF16, kind=SK("qaug"))
    kaug = P.dram("kaug", [8, 97, T], BF16, kind=SK("kaug"))

    PS = [P.ps([128, 512], F32, name=f"psb{i}") for i in range(6)]
    PT = [P.ps([128, 1024], BF16, name=f"ptb{i}") for i in range(2)]
    identb = P.sb([128, 128], BF16, name="identb_s")
    identf = P.sb([128, 128], F32, name="identf_s")
    onesf = P.sb([128, 128], F32, name="onesf_s")
    sil = P.sb([128, 8, 2], F32, name="sil")
    epsr = P.sb([128, 4], F32, name="epsr")
    kb.dma(identb, identb[:, :], I["identb"], I["identb"].ap)
    kb.dma(identf, identf[:, :], I["identf"], I["identf"].ap)
    kb.dma(onesf, onesf[:, :], I["onesf"], I["onesf"].ap)
    cv = P.sb([128, 16], F32, name="cv")
    kb.dma(cv, cv[:, :], I["cvec"], I["cvec"].ap)
    kb.act(sil, sil[:, :, :], cv, cv[:, :].rearrange("p (k w) -> p k w", w=2), AF.Silu)
    kb.memset(V, epsr, epsr[:, 0:1], RMS_EPS)
    kb.memset(V, epsr, epsr[:, 1:2], RWKV_EPS)
    kb.memset(V, epsr, epsr[:, 2:3], RET_EPS)
    kb.memset(V, epsr, epsr[:, 3:4], 1e-24)
    MOD = []

    def rowb(l, j0, w, n=8):
        return modd[l].ap.rearrange("(j w) p -> w j p", w=2)[w, j0:j0 + n, :].partition_broadcast(128)

    def phase_mod(l):
        kb.reset()
        wmb = [kb.alloc([128, 8, 512], F32, "wm") for _ in range(2)]
        bm = kb.alloc([128, 48], F32, "bm")
        nm = kb.alloc([128, 16], F32, "nm")
        pk = kb.alloc([128, 128], F32, "pk")
        pkT = kb.alloc([128, 128], F32, "pkT")
        kb.dma(bm, bm[:, :], I[f"bmod{l}"], I[f"bmod{l}"].ap)
        kb.dma(nm, nm[:, 0:8], I[f"nmix{l}"], I[f"nmix{l}"].ap)
        kb.dma(nm, nm[:, 8:16], I[f"nffn{l}"], I[f"nffn{l}"].ap)
        psm = PS[0]
        wsrc = I[f"wmod{l}"].ap.rearrange("(k p) n -> p k n", p=128)
        for cb in range(12):
            wm = wmb[cb % 2]
            kb.dma(wm, wm[:, :, :], I[f"wmod{l}"], wsrc[:, :, cb * 512:(cb + 1) * 512])
            for sub in range(4):
                j = cb * 4 + sub
                for k in range(8):
                    kb.mm(psm, psm[:, 2 * j:2 * j + 2], wm, wm[:, k, sub * 128:(sub + 1) * 128], sil, sil[:, k, :], k == 0, k == 7)
        vec = P.sb([128, 4, 8, 2], F32, name=f"vec{l}")
        kb.memset(V, pk, pk[:, :], 0.0)
        pk3 = pk[:, 0:96].rearrange("p (j w) -> p j w", w=2)
        kb.tt(V, pk, pk3, psm, psm[:, 0:96].rearrange("p (j w) -> p j w", w=2), bm,
              bm[:, :].unsqueeze(2).broadcast_to([128, 48, 2]), ALU.add)
        for (ai, scj, shj, nmo) in ((0, 8, 0, 0), (2, 32, 24, 8)):
            kb.ts(V, vec, vec[:, ai, :, :], pk, pk3[:, scj:scj + 8, :], 1.0, None, ALU.add)
            kb.tt(V, vec, vec[:, ai, :, :], vec, vec[:, ai, :, :], nm,
                  nm[:, nmo:nmo + 8].unsqueeze(2).broadcast_to([128, 8, 2]), ALU.mult)
            kb.cp(V, vec, vec[:, ai + 1, :, :], pk, pk3[:, shj:shj + 8, :])
        kb.cp(V, pk, pk[:, 96:112], vec, vec[:, 2, :, :].rearrange("p k w -> p (k w)"))
        pst = PS[1]
        kb.tr(pst, pst[:, 0:128], pk, pk[:, :], identf, identf[:, :])
        kb.cp(V, pkT, pkT[:, :], pst, pst[:, 0:128])
        kb.dma(modd[l], modd[l].ap, pkT, pkT[:, :])
        MOD.append(vec)

    class NormCtx:
        def __init__(self):
            self.ht = [kb.alloc([128, D], F32, "ht") for _ in range(2)]
            self.junk = kb.alloc([128, D], F32, "junk")
            self.hnb = [kb.alloc([128, D], BF16, "hnb") for _ in range(2)]
            self.st = [kb.alloc([128, 4], F32, "st") for _ in range(2)]
            self.n = 0

    def norm_tile(ncx, i, src, vec, ai, dst_t, dst, keep_hn=None):
        w = 1 if i < 2 else 0
        b = ncx.n % 2
        ncx.n += 1
        ht, hnb, st = ncx.ht[b], ncx.hnb[b], ncx.st[b]
        kb.dma(ht, ht[:, :], src, src[i * 128:(i + 1) * 128, :])
        kb.act(ncx.junk, ncx.junk[:, :], ht, ht[:, :], AF.Square, accum=st[:, 0:1], writes=[st])
        kb.act(st, st[:, 1:2], st, st[:, 0:1], AF.Sqrt, scale=1.0 / D, bias=epsr[:, 0:1], reads=[epsr])
        kb.recip(st, st[:, 2:3], st, st[:, 1:2])
        if keep_hn is not None:
            kb.ts(V, keep_hn, keep_hn[:, :], ht, ht[:, :], st[:, 2:3], None, ALU.mult, reads=[st])
        kb.act(hnb, hnb[:, :], ht, ht[:, :], AF.Copy, scale=st[:, 2:3], reads=[st])
        pt = PT[ncx.n % 2]
        pt3 = pt[:, :].rearrange("p (c t) -> p c t", c=8)
        for c in range(8):
            kb.tr(pt, pt3[:, c, :], hnb, hnb[:, c * 128:(c + 1) * 128], identb, identb[:, :])
        for c in range(8):
            if c % 2 == 0:
                kb.ts(V, dst_t, dst[:, c, :], pt, pt3[:, c, :], vec[:, ai, c, w:w + 1], vec[:, ai + 1, c, w:w + 1],
                      ALU.mult, ALU.add, reads=[vec])
            else:
                kb.act(dst_t, dst[:, c, :], pt, pt3[:, c, :], AF.Identity, scale=vec[:, ai, c, w:w + 1],
                       bias=vec[:, ai + 1, c, w:w + 1], reads=[vec])
        return ht, st

    hm_mark = [0]

    def phase_norm_all(l, src, vec, ai):
        hm = [kb.alloc([128, 8, n], BF16, f"hm{tt}") for tt, (t0, n) in enumerate(TT512)]
        hm_mark[0] = kb.off
        ncx = NormCtx()
        for i in range(NT):
            tok = i * 128
            tt = 0 if tok < 256 else 1 + (tok - 256) // 512
            o = tok - TT512[tt][0]
            norm_tile(ncx, i, src, vec, ai, hm[tt], hm[tt][:, :, o:o + 128])
        return hm

    def phase_proj(l, hm):
        wsrc = I[f"win{l}"].ap.rearrange("(k p) n -> p k n", p=128)
        wtb = [kb.alloc([128, 8, 512], BF16, "wt") for _ in range(2)]
        obs = [kb.alloc([128, 512], BF16, "ob") for _ in range(4)]
        nblk = (UROWS + 511) // 512
        import os
        nblk = int(os.environ.get('PROJ_NBLK', nblk))
        cnt = 0
        for cb in range(nblk):
            c0 = U0 + cb * 512
            ncol = min(512, NCOL - c0)
            wt = wtb[cb % 2]
            kb.dma(wt, wt[:, :, 0:ncol], I[f"win{l}"], wsrc[:, :, c0:c0 + ncol], q=G)
            for sub in range((ncol + 127) // 128):
                m = min(128, ncol - sub * 128)
                r0 = c0 - U0 + sub * 128
                for tt, (t0, n) in enumerate(TT512):
                    ps = PS[cnt % 4]
                    ob = obs[cnt % 4]
                    cnt += 1
                    for k in range(8):
                        kb.mm(ps, ps[0:m, 0:n], wt, wt[:, k, sub * 128:sub * 128 + m], hm[tt], hm[tt][:, k, 0:n], k == 0, k == 7)
                    kb.cp(kb.evac_eng(), ob, ob[0:m, 0:n], ps, ps[0:m, 0:n])
                    kb.store(uT, uT[r0:r0 + m, t0:t0 + n], ob, ob[0:m, 0:n], q=SY)

    def phase_rwkv_pre(l, hm):
        wsrc = I[f"win{l}"].ap.rearrange("(k p) n -> p k n", p=128)
        W1 = kb.alloc([128, 8, 1920], BF16, "W1")
        kb.dma(W1, W1[:, :, :], I[f"win{l}"], wsrc[:, :, 0:1920], q=G)
        m1 = kb.alloc([128, 1920], F32, "m1")
        m2 = kb.alloc([128, 1920], F32, "m2")
        kb.dma(m2, m2[:, :], I[f"mu{l}"], I[f"mu{l}"].ap[0, :].partition_broadcast(128))
        kb.ts(V, m1, m1[:, :], m2, m2[:, :], -1.0, 1.0, ALU.mult, ALU.add)
        kb.ts(V, m2, m2[:, :], m2, m2[:, :], 0.5, None, ALU.mult)
        hsb = [kb.alloc([128, 8, 128], BF16, "hs") for _ in range(2)]
        sh2b = [kb.alloc([128, 512], F32, "sh2") for _ in range(2)]
        rkvb = [kb.alloc([128, 1920], F32, "rkv") for _ in range(2)]

        def hm_slice(k_lo, k_hi):
            res = []
            t = k_lo
            while t < k_hi:
                tt = 0 if t < 256 else 1 + (t - 256) // 512
                t0, n = TT512[tt]
                e = min(k_hi, t0 + n)
                res.append((tt, t - t0, e - t))
                t = e
            return res
        cnt = 0
        for i in range(NT):
            hs, rkv = hsb[i % 2], rkvb[i % 2]
            tok = i * 128
            s0, s1 = (0, 256) if i < 2 else (256, T)
            kb.memset(G, hs, hs[:, :, :], 0.0)
            a, b = max(tok - 1, s0), min(tok + 127, s1)
            for (tt, o, n) in hm_slice(a, b):
                dst0 = (TT512[tt][0] + o + 1) - tok
                kb.cp(G, hs, hs[:, :, dst0:dst0 + n], hm[tt], hm[tt][:, :, o:o + n])
            a, b = max(tok + 1, s0), min(tok + 129, s1)
            for (tt, o, n) in hm_slice(a, b):
                dst0 = (TT512[tt][0] + o - 1) - tok
                kb.tt(G, hs, hs[:, :, dst0:dst0 + n], hs, hs[:, :, dst0:dst0 + n], hm[tt], hm[tt][:, :, o:o + n], ALU.add)
            (tt, o, n), = hm_slice(tok, tok + 128)
            for cb in range(4):
                c0 = cb * 512
                ncol = min(512, 1920 - c0)
                ps, ps2 = PS[(cnt % 3) * 2], PS[(cnt % 3) * 2 + 1]
                sh2 = sh2b[cnt % 2]
                cnt += 1
                for k in range(8):
                    kb.mm(ps, ps[:, 0:ncol], hm[tt], hm[tt][:, k, o:o + 128], W1, W1[:, k, c0:c0 + ncol], k == 0, k == 7)
                for k in range(8):
                    kb.mm(ps2, ps2[:, 0:ncol], hs, hs[:, k, :], W1, W1[:, k, c0:c0 + ncol], k == 0, k == 7)
                kb.tt(V, rkv, rkv[:, c0:c0 + ncol], ps, ps[:, 0:ncol], m1, m1[:, c0:c0 + ncol], ALU.mult)
                kb.tt(V, sh2, sh2[:, 0:ncol], ps2, ps2[:, 0:ncol], m2, m2[:, c0:c0 + ncol], ALU.mult)
                kb.tt(G, rkv, rkv[:, c0:c0 + ncol], rkv, rkv[:, c0:c0 + ncol], sh2, sh2[:, 0:ncol], ALU.add)
            kb.store(rkvd, rkvd[tok:tok + 128, :], rkv, rkv[:, :], q=G)

    def phase_rwkv(l):
        kb.reset()
        F32R = mybir.dt.float32r
        lw = kb.alloc([128, 3, 512], BF16, "lw")
        for j, nmn in enumerate(("w2", "a2", "g2")):
            kb.dma(lw, lw[:, j, :], I[f"{nmn}{l}"], I[f"{nmn}{l}"].ap, q=G)
        pv = kb.alloc([128, 7, 512], F32, "pv")
        for d in range(2):
            kb.dma(pv, pv[:, d, :], I[f"w0{l}"], I[f"w0{l}"].ap[d, :].partition_broadcast(128))
            kb.dma(pv, pv[:, 2 + d, :], I[f"a0{l}"], I[f"a0{l}"].ap[d, :].partition_broadcast(128))
        for j in range(3):
            kb.dma(pv, pv[:, 4 + j, :], I[f"rvec{l}"], I[f"rvec{l}"].ap[j, :].partition_broadcast(128))
        cst = kb.alloc([128, 256], F32, "cst")
        cmk = kb.alloc([128, 1280], BF16, "cmk")
        cm2 = kb.alloc([128, 896], BF16, "cm2")
        kb.dma(cst, cst[:, 0:256], I["tri"], I["tri"].ap)
        kb.dma(cmk, cmk[:, 0:1024], I["mask4"], I["mask4"].ap)
        kb.dma(cmk, cmk[:, 1024:1280], I["maskL"], I["maskL"].ap)
        kb.dma(cm2, cm2[:, :], I["invmask"], I["invmask"].ap)
        tri = cst
        order = {0: [0, 1] + list(range(2, NT)), 1: [1, 0] + list(range(NT - 1, 1, -1))}
        v8 = lambda ap: ap.rearrange("p (h n) -> p h n", h=8)

        class Bufs:
            pass
        BD = []
        for d in range(2):
            b = Bufs()
            b.d = d
            b.H32 = kb.alloc([128, 4, 64], F32, "H32")
            b.Hb = kb.alloc([128, 4, 64], BF16, "Hb")
            kb.memset(V, b.H32, b.H32[:, :, :], 0.0)
            kb.memset(V, b.Hb, b.Hb[:, :, :], 0.0)
            b.rkv = kb.alloc([128, 1920], F32, "rkv")
            b.lo = kb.alloc([128, 384], BF16, "lo")
            b.loT = kb.alloc([128, 3, 128], BF16, "loT")
            for nmn in ("kk", "t1", "t2", "t3", "aa", "kd", "lgw"):
                setattr(b, nmn, kb.alloc([128, 512], F32, nmn))
            b.lP = b.aa
            b.Ot = b.t3
            b.sm = kb.alloc([128, 32], F32, "sm")
            b.TM = kb.alloc([128, 4, 512], BF16, "TM")
            b.Bt = [kb.alloc([128, 512], BF16, "Bt") for _ in range(2)]
            b.Kt = [kb.alloc([128, 512], BF16, "Kt") for _ in range(2)]
            b.Vb = [kb.alloc([128, 512], BF16, "Vb") for _ in range(2)]
            b.FM = [kb.alloc([128, 4, 4, 128], BF16, "FM") for _ in range(2)]
            b.GM = [kb.alloc([128, 8, 384], BF16, "GM") for _ in range(2)]
            b.Otb = kb.alloc([128, 512], F32, "Otb")
            b.inv = []
            for g in range(2):
                iv = Bufs()
                for nmn in ("Xf", "Yf"):
                    setattr(iv, nmn, [kb.alloc([128, 4, 128], F32, nmn) for _ in range(2)])
                for nmn in ("Nb", "NTb", "Eb", "ETb", "P1b", "P2b"):
                    setattr(iv, nmn, kb.alloc([128, 4, 128], BF16, nmn))
                b.inv.append(iv)
            b.Wk = kb.alloc([128, 8, 128], BF16, "Wk")
            b.Gs = kb.alloc([128, 512], BF16, "Gs")
            b.Us = kb.alloc([128, 512], BF16, "Us")
            b.PCc = [kb.alloc([128, 4], F32, "PCc") for _ in range(2)]
            BD.append(b)

        def shared(b, i, par=0):
            rkv, t1, t2, sm, kk = b.rkv, b.t1, b.t2, b.sm, b.kk
            kb.dma(rkv, rkv[:, :], rkvd, rkvd[i * 128:(i + 1) * 128, :])
            yield
            kb.tt(G, t1, t1[:, :], rkv, rkv[:, 512:1024], pv, pv[:, 4, :], ALU.mult)
            yield
            kb.tt(G, t2, t2[:, :], t1, t1[:, :], t1, t1[:, :], ALU.mult)
            yield
            kb.red(sm, sm[:, 0:8], t2, v8(t2[:, :]))
            yield
            kb.act(sm, sm[:, 8:16], sm, sm[:, 0:8], AF.Sqrt)
            yield
            kb.ts(V, sm, sm[:, 8:16], sm, sm[:, 8:16], 1e-12, None, ALU.max)
            yield
            kb.recip(sm, sm[:, 16:24], sm, sm[:, 8:16])
            yield
            kb.tt(V, kk, v8(kk[:, :]), t1, v8(t1[:, :]), sm, sm[:, 16:24].unsqueeze(2).broadcast_to([128, 8, 64]), ALU.mult)
            yield
            kb.cp(G, b.Vb[par], b.Vb[par][:, :], rkv, rkv[:, 1024:1536])
            yield
            kb.act(b.lo, b.lo[:, 0:128], rkv, rkv[:, 1536:1664], AF.Tanh)
            yield
            kb.cp(G, b.lo, b.lo[:, 128:256], rkv, rkv[:, 1664:1792])
            yield
            kb.act(b.lo, b.lo[:, 256:384], rkv, rkv[:, 1792:1920], AF.Sigmoid)
            yield
            pt = PT[b.d]
            for j in range(3):
                kb.tr(pt, pt[:, j * 128:(j + 1) * 128], b.lo, b.lo[:, j * 128:(j + 1) * 128], identb, identb[:, :])
            kb.cp(S, b.loT, b.loT[:, :, :], pt, pt[:, 0:384].rearrange("p (j t) -> p j t", j=3))
            yield

        def akd(b, d, pa):
            o64 = d * 64
            kb.mm(pa, pa[:, :], b.loT, b.loT[o64:o64 + 64, 1, :], lw, lw[o64:o64 + 64, 1, :])
            yield
            kb.tt(V, b.t2, b.t2[:, :], pa, pa[:, :], pv, pv[:, 2 + d, :], ALU.add)
            yield
            kb.act(b.aa, b.aa[:, :], b.t2, b.t2[:, :], AF.Sigmoid)
            yield
            kb.ts(G, b.t2, b.t2[:, :], b.aa, b.aa[:, :], 1.0, -1.0, ALU.mult, ALU.add)
            yield
            kb.tt(G, b.t2, b.t2[:, :], b.t2, b.t2[:, :], pv, pv[:, 5, :], ALU.mult)
            yield
            kb.ts(G, b.t2, b.t2[:, :], b.t2, b.t2[:, :], 1.0, 1.0, ALU.mult, ALU.add)
            yield
            kb.tt(G, b.kd, b.kd[:, :], b.t2, b.t2[:, :], b.rkv, b.rkv[:, 512:1024], ALU.mult)
            yield


        def inverse(chains, par):
            identg = identf[:, :].unsqueeze(1).broadcast_to([128, 4, 128])

            def mk(j):
                return cm2[:, j * 128:(j + 1) * 128].unsqueeze(1).broadcast_to([128, 4, 128])
            v4 = lambda ps: ps[:, :].rearrange("p (a t) -> p a t", a=4)

            def mm4(ps, l_t, r_t):
                for hi in range(4):
                    kb.mm(ps, ps[:, hi * 128:(hi + 1) * 128], l_t, l_t[:, hi, :], r_t, r_t[:, hi, :])
            for (iv, bk) in chains:
                kb.tt(G, iv.P1b, iv.P1b[:, :, :], iv.Xf[par], iv.Xf[par][:, :, :], cm2, mk(0), ALU.mult)
                kb.tt(G, iv.P2b, iv.P2b[:, :, :], iv.Yf[par], iv.Yf[par][:, :, :], cm2, mk(0), ALU.mult)
                kb.tt(V, iv.Nb, iv.Nb[:, :, :], iv.P1b, iv.P1b[:, :, :], identf, identg, ALU.add)
                kb.tt(V, iv.NTb, iv.NTb[:, :, :], iv.P2b, iv.P2b[:, :, :], identf, identg, ALU.add)
                yield
            NLEV = 6
            for lev in range(NLEV):
                for (iv, bk) in chains:
                    kb.tt(G, iv.Eb, iv.Eb[:, :, :], iv.Xf[par], iv.Xf[par][:, :, :], cm2, mk(1 + lev), ALU.mult)
                    kb.tt(G, iv.ETb, iv.ETb[:, :, :], iv.Yf[par], iv.Yf[par][:, :, :], cm2, mk(1 + lev), ALU.mult)
                    yield
                for (iv, (pa_, pat_)) in chains:
                    mm4(pa_, iv.ETb, iv.Nb)
                    mm4(pat_, iv.Eb, iv.NTb)
                    kb.cp(S, iv.P1b, iv.P1b[:, :, :], pa_, v4(pa_))
                    kb.cp(S, iv.P2b, iv.P2b[:, :, :], pat_, v4(pat_))
                    yield
                for (iv, (pa_, pat_)) in chains:
                    mm4(pa_, iv.NTb, iv.P1b)
                    mm4(pat_, iv.Nb, iv.P2b)
                    kb.tt(V, iv.Nb, iv.Nb[:, :, :], pa_, v4(pa_), iv.Nb, iv.Nb[:, :, :], ALU.add)
                    kb.tt(V, iv.NTb, iv.NTb[:, :, :], pat_, v4(pat_), iv.NTb, iv.NTb[:, :, :], ALU.add)
                    yield

        def direction(i, d, par):
            b = BD[d]
            rkv, kk, t1, t2, t3, aa, kd, lgw, lP, sm = b.rkv, b.kk, b.t1, b.t2, b.t3, b.aa, b.kd, b.lgw, b.lP, b.sm
            TM, Bt, Kt, Vb, FM, GM, Wk, Gs, Us, Ot, PCc, H32, Hb = b.TM, b.Bt[par], b.Kt[par], b.Vb[par], b.FM[par], b.GM[par], b.Wk, b.Gs, b.Us, b.Ot, b.PCc[par], b.H32, b.Hb
            trid = tri[:, d * 128:(d + 1) * 128]
            m4 = cmk[:, d * 512:(d + 1) * 512]
            mL = cmk[:, 1024 + d * 128:1024 + (d + 1) * 128]
            o64 = d * 64
            pz = pa = PS[4 + d]
            kb.mm(pz, pz[:, :], b.loT, b.loT[o64:o64 + 64, 0, :], lw, lw[o64:o64 + 64, 0, :])
            yield
            kb.tt(V, t1, t1[:, :], pz, pz[:, :], pv, pv[:, d, :], ALU.add)
            yield
            kb.act(t1, t1[:, :], t1, t1[:, :], AF.Sigmoid)
            yield
            kb.ts(G, lgw, lgw[:, :], t1, t1[:, :], -0.6065306597126334, 0.0, ALU.mult, ALU.add)
            yield
            yield from akd(b, d, pa)
            kb.tt(G, t3, t3[:, :], kk, kk[:, :], aa, aa[:, :], ALU.mult)
            pl = pc = PS[4 + d]
            kb.mm(pl, pl[:, :], tri, trid, lgw, lgw[:, :])
            yield
            kb.cp(S, lP, lP[:, :], pl, pl[:, :])
            yield
            kb.mm(pc, pc[:, :], onesf, onesf[:, :], lgw, lgw[:, :])
            yield
            kb.tt(V, t1, t1[:, :], lP, lP[:, :], lgw, lgw[:, :], ALU.subtract)
            yield
            kb.act(t1, t1[:, :], t1, t1[:, :], AF.Exp)
            yield
            kb.stt(TM, TM[:, 0, :], t1, t1[:, :], -1.0, kk, kk[:, :], ALU.mult, ALU.mult)
            yield
            kb.act(t1, t1[:, :], lP, lP[:, :], AF.Exp)
            yield
            kb.tt(V, TM, TM[:, 1, :], t1, t1[:, :], rkv, rkv[:, 0:512], ALU.mult)
            yield
            kb.act(t1, t1[:, :], lP, lP[:, :], AF.Exp, scale=-1.0)
            yield
            kb.tt(V, TM, TM[:, 2, :], t3, t3[:, :], t1, t1[:, :], ALU.mult)
            yield
            kb.tt(G, TM, TM[:, 3, :], kd, kd[:, :], t1, t1[:, :], ALU.mult)
            yield
            kb.tt(V, t2, t2[:, :], pc, pc[:, :], lP, lP[:, :], ALU.subtract)
            yield
            kb.act(t2, t2[:, :], t2, t2[:, :], AF.Exp)
            yield
            kb.tt(V, Bt, Bt[:, :], t3, t3[:, :], t2, t2[:, :], ALU.mult)
            yield
            kb.tt(G, Kt, Kt[:, :], kd, kd[:, :], t2, t2[:, :], ALU.mult)
            yield
            pcc = PS[4 + d]
            for p in range(4):
                kb.mm(pcc, pcc[:, 2 * p:2 * p + 2], lgw, lgw[:, p * 128:(p + 1) * 128], onesf, onesf[:, 0:2])
            kb.act(PCc, PCc[:, :], pcc, pcc[:, 0:8].rearrange("p (a b) -> p a b", b=2)[:, :, 0], AF.Exp)
            yield
            for q in range(4):
                pt = PT[d]
                for p in range(4):
                    kb.tr(pt, pt[:, p * 128:(p + 1) * 128], TM, TM[:, q, p * 128:(p + 1) * 128], identb, identb[:, :])
                kb.cp(kb.evac_eng(), FM, FM[:, :, q, :], pt, pt[:, 0:512].rearrange("p (a t) -> p a t", a=4))
                yield
            for h in range(8):
                g, hi = h // 4, h % 4
                iv = b.inv[g]
                p, o = h // 2, (h % 2) * 64
                pg = PS[h % 2]
                rhsAR = FM[o:o + 64, p, 0:2, :]
                kb.mm(pg, pg[:, 0:256], FM, FM[o:o + 64, p, 2, :], FM, rhsAR)
                kb.mm(pg, pg[:, 256:512], FM, FM[o:o + 64, p, 3, :], FM, rhsAR)
                kb.tt(V, iv.Yf[par], iv.Yf[par][:, hi, :], pg, pg[:, 0:128], cmk, m4[:, 0:128], ALU.mult)
                kb.tt(V, GM, GM[:, h, :], pg, pg[:, 128:512], cmk, m4[:, 128:512], ALU.mult)
                pg2 = PS[2 + h % 2]
                kb.mm(pg2, pg2[:, 0:128], FM, FM[o:o + 64, p, 0, :], FM, FM[o:o + 64, p, 2, :])
                kb.tt(V, iv.Xf[par], iv.Xf[par][:, hi, :], pg2, pg2[:, 0:128], cmk, mL, ALU.mult)
                yield
            return

        def post(i, d, par):
            b = BD[d]
            rkv, kk, t1, t2, t3, aa, kd, lgw, lP, sm = b.rkv, b.kk, b.t1, b.t2, b.t3, b.aa, b.kd, b.lgw, b.lP, b.sm
            TM, Bt, Kt, Vb, FM, GM, Wk, Gs, Us, Ot, PCc, H32, Hb = b.TM, b.Bt[par], b.Kt[par], b.Vb[par], b.FM[par], b.GM[par], b.Wk, b.Gs, b.Us, b.Otb, b.PCc[par], b.H32, b.Hb
            pG, pU, pO, pH = (PS[0], PS[1], PS[2], PS[3]) if d == 0 else (PS[4], PS[5], PS[4], PS[5])
            for h in range(8):
                p, o = h // 2, (h % 2) * 64
                kb.mm(pG, pG[:, h * 64:(h + 1) * 64], FM, FM[o:o + 64, p, 0, :], Hb, Hb[o:o + 64, p, :], True, False)
                kb.mm(pG, pG[:, h * 64:(h + 1) * 64], GM, GM[:, h, 128:256], Vb, Vb[:, h * 64:(h + 1) * 64], False, True)
            kb.cp(S, Gs, Gs[:, :], pG, pG[:, :])
            yield
            for h in range(8):
                kb.mm(pU, pU[:, h * 64:(h + 1) * 64], b.inv[h // 4].NTb, b.inv[h // 4].NTb[:, h % 4, :], Gs, Gs[:, h * 64:(h + 1) * 64])
            kb.cp(S, Us, Us[:, :], pU, pU[:, :])
            yield
            for h in range(8):
                p, o = h // 2, (h % 2) * 64
                sl = slice(h * 64, (h + 1) * 64)
                kb.mm(pO, pO[:, sl], FM, FM[o:o + 64, p, 1, :], Hb, Hb[o:o + 64, p, :], True, False)
                kb.mm(pO, pO[:, sl], GM, GM[:, h, 0:128], Us, Us[:, sl], False, False)
                kb.mm(pO, pO[:, sl], GM, GM[:, h, 256:384], Vb, Vb[:, sl], False, True)
            kb.cp(S, Ot, Ot[:, :], pO, pO[:, :])
            yield
            dst = ofw if d == 0 else obw
            kb.store(dst, dst[i * 128:(i + 1) * 128, :], Ot, Ot[:, :], q=S)
            yield
            for h in range(8):
                p, o = h // 2, (h % 2) * 64
                sl = slice(h * 64, (h + 1) * 64)
                kb.mm(pH, pH[o:o + 64, p * 64:(p + 1) * 64], Bt, Bt[:, sl], Us, Us[:, sl], True, False)
                kb.mm(pH, pH[o:o + 64, p * 64:(p + 1) * 64], Kt, Kt[:, sl], Vb, Vb[:, sl], False, True)
            for p in range(4):
                kb.stt(H32, H32[:, p, :], H32, H32[:, p, :], PCc[:, p:p + 1], pH, pH[:, p * 64:(p + 1) * 64],
                       ALU.mult, ALU.add, reads=[PCc])
            kb.cp(V, Hb, Hb[:, :, :], H32, H32[:, :, :])
            yield

        banks = [(PS[0], PS[1]), (PS[2], PS[3])]

        def run(gens, first_weight=1):
            gens = list(gens)
            lead = gens[0] if gens else None
            while gens:
                for gn in list(gens):
                    try:
                        for _ in range(first_weight if gn is lead else 1):
                            next(gn)
                    except StopIteration:
                        gens.remove(gn)

        def E(n, d):
            yield from shared(BD[d], order[d][n], n % 2)
            yield from direction(order[d][n], d, n % 2)
        run([E(0, 0), E(0, 1)])
        for n in range(NT):
            gens = [inverse([(BD[d].inv[g], banks[g]) for g in range(2) for d in range(2)], n % 2)]
            if n + 1 < NT:
                gens += [E(n + 1, 0), E(n + 1, 1)]
            run(gens, RUNW)
            run([post(order[0][n], 0, n % 2), post(order[1][n], 1, n % 2)])
        P.barrier()
        b = BD[0]
        rkv, t1, t2, sm, aa, kd = b.rkv, b.t1, b.t2, b.sm, b.aa, b.kd
        bsum = b.PCc[0]
        lnp = BD[1].rkv
        ob2 = BD[1].t1
        Ot = BD[1].t2
        yb = BD[1].Gs
        yo = BD[1].Us
        bs8 = BD[1].sm
        for j in range(2):
            kb.dma(lnp, lnp[:, j * 512:(j + 1) * 512], I[f"rvec{l}"], I[f"rvec{l}"].ap[3 + j, :].partition_broadcast(128))
        for i in range(2 if l == n_layers - 1 else 0, NT):
            for _ in shared(b, i):
                pass
            for d in range(2):
                for _ in akd(b, d, PS[5]):
                    pass
                kb.tt(V, t2, t2[:, :], kd, kd[:, :], pv, pv[:, 6, :], ALU.mult)
                kb.tt(V, t2, t2[:, :], t2, t2[:, :], rkv, rkv[:, 0:512], ALU.mult)
                kb.red(sm, sm[:, 24:32], t2, v8(t2[:, :]))
                if d == 0:
                    kb.cp(V, bs8, bs8[:, 0:8], sm, sm[:, 24:32])
                else:
                    kb.tt(V, bs8, bs8[:, 0:8], bs8, bs8[:, 0:8], sm, sm[:, 24:32], ALU.add)
            kb.dma(Ot, Ot[:, :], ofw, ofw[i * 128:(i + 1) * 128, :])
            kb.dma(ob2, ob2[:, :], obw, obw[i * 128:(i + 1) * 128, :])
            kb.tt(V, t1, t1[:, :], Ot, Ot[:, :], ob2, ob2[:, :], ALU.add)
            kb.red(sm, sm[:, 0:8], t1, v8(t1[:, :]))
            kb.ts(V, sm, sm[:, 0:8], sm, sm[:, 0:8], 1.0 / 64, None, ALU.mult)
            kb.tt(V, t1, v8(t1[:, :]), t1, v8(t1[:, :]), sm, sm[:, 0:8].unsqueeze(2).broadcast_to([128, 8, 64]), ALU.subtract)
            kb.tt(G, t2, t2[:, :], t1, t1[:, :], t1, t1[:, :], ALU.mult)
            kb.red(sm, sm[:, 8:16], t2, v8(t2[:, :]))
            kb.act(sm, sm[:, 8:16], sm, sm[:, 8:16], AF.Sqrt, scale=1.0 / 64, bias=epsr[:, 1:2], reads=[epsr])
            kb.recip(sm, sm[:, 16:24], sm, sm[:, 8:16])
            kb.tt(V, t1, v8(t1[:, :]), t1, v8(t1[:, :]), sm, sm[:, 16:24].unsqueeze(2).broadcast_to([128, 8, 64]), ALU.mult)
            kb.tt(G, t1, t1[:, :], t1, t1[:, :], lnp, lnp[:, 0:512], ALU.mult)
            kb.tt(G, t1, t1[:, :], t1, t1[:, :], lnp, lnp[:, 512:1024], ALU.add)
            kb.tt(V, t2, v8(t2[:, :]), rkv, v8(rkv[:, 1024:1536]), bs8, bs8[:, 0:8].unsqueeze(2).broadcast_to([128, 8, 64]), ALU.mult)
            kb.tt(V, t1, t1[:, :], t1, t1[:, :], t2, t2[:, :], ALU.add)
            pg = PS[4]
            kb.mm(pg, pg[:, :], b.loT, b.loT[:, 2, :], lw, lw[:, 2, :])
            kb.tt(V, yb, yb[:, :], pg, pg[:, :], t1, t1[:, :], ALU.mult)
            pt = PT[1]
            for c in range(4):
                kb.tr(pt, pt[:, c * 128:(c + 1) * 128], yb, yb[:, c * 128:(c + 1) * 128], identb, identb[:, :])
            kb.cp(S, yo, yo[:, :], pt, pt[:, 0:512])
            kb.store(yT, yT[0:512, i * 128:(i + 1) * 128].rearrange("(c p) t -> p c t", p=128), yo, yo[:, :].rearrange("p (c t) -> p c t", c=4), q=S)

    def phase_ret(l):
        kb.reset()
        rc = kb.alloc([128, 384], F32, "rc")
        kb.dma(rc, rc[:, :], I["retc"], I["retc"].ap)
        rt = kb.alloc([128, 4], F32, "rt")
        kb.dma(rt, rt[:, :], I["rett"], I["rett"].ap)
        rrow = kb.alloc([128, 256], F32, "rrow")
        kb.dma(rrow, rrow[:, :], I["retrow"], I["retrow"].ap)
        dec = kb.alloc([128, 8], F32, "dec")
        kb.dma(dec, dec[:, :], I[f"rdec{l}"], I[f"rdec{l}"].ap[0, :].partition_broadcast(128))
        lg = kb.alloc([128, 8], F32, "lg")
        kb.act(lg, lg[:, :], dec, dec[:, :], AF.Exp)
        kb.ts(V, lg, lg[:, :], lg, lg[:, :], -1.0, None, ALU.mult)
        DM = kb.alloc([128, 8, 128], F32, "DM")
        xi = kb.alloc([128, 8, 128], F32, "xi")
        zt = kb.alloc([128, 8], F32, "zt")
        gck = kb.alloc([128, 8], F32, "gck")
        for d in range(2):
            for h in range(4):
                dh = d * 4 + h
                kb.act(DM, DM[:, dh, :], rc, rc[:, 0:128], AF.Exp, scale=lg[:, dh:dh + 1], reads=[lg])
                kb.tt(V, DM, DM[:, dh, :], DM, DM[:, dh, :], rc, rc[:, 128 + d * 128:256 + d * 128], ALU.mult)
                kb.act(xi, xi[:, dh, :], rrow, rrow[:, d * 128:(d + 1) * 128], AF.Exp, scale=lg[:, dh:dh + 1], reads=[lg])
                kb.act(zt, zt[:, dh:dh + 1], rt, rt[:, 2 + d:3 + d], AF.Exp, scale=lg[:, dh:dh + 1], reads=[lg])
        kb.act(gck, gck[:, :], lg, lg[:, :], AF.Exp, scale=128.0)
        rln = kb.alloc([128, 8], F32, "rln")
        kb.dma(rln, rln[:, :], I[f"rln{l}"], I[f"rln{l}"].ap)
        R32 = kb.alloc([128, 4, 128], F32, "R32")
        Rb = kb.alloc([128, 4, 128], BF16, "Rb")
        kb.memset(V, R32, R32[:, :, :], 0.0)
        kb.memset(V, Rb, Rb[:, :, :], 0.0)
        raw = kb.alloc([128, 12, 128], BF16, "raw")
        rope = kb.alloc([128, 2, 128], BF16, "rope")
        qk = kb.alloc([128, 4, 128], F32, "qk")
        tmp = kb.alloc([128, 4, 128], F32, "tmp")
        qkb = kb.alloc([128, 4, 128], BF16, "qkb")
        qx = kb.alloc([128, 2, 128], BF16, "qx")
        ktm = kb.alloc([128, 4, 64], BF16, "ktm")
        ktr = kb.alloc([128, 4, 64], BF16, "ktr")
        vtm = kb.alloc([128, 512], BF16, "vtm")
        PTt = kb.alloc([128, 4, 128], BF16, "PTt")
        yt = kb.alloc([128, 512], F32, "yt")
        y2 = kb.alloc([128, 512], F32, "y2")
        ysq = kb.alloc([128, 512], F32, "ysq")
        ybf = kb.alloc([128, 512], BF16, "ybf")
        smr = kb.alloc([128, 16], F32, "smr")
        gt = kb.alloc([128, 4, 128], BF16, "gt")
        gs = kb.alloc([128, 4, 128], F32, "gs")
        yo = kb.alloc([128, 4, 128], BF16, "yo")
        order = {0: [0, 1] + list(range(2, NT)), 1: [1, 0] + list(range(NT - 1, 1, -1))}

        def load_tile(i):
            sl = slice(i * 128, (i + 1) * 128)
            kb.dma(raw, raw[:, :, :], uT, uT[0:1536, sl].rearrange("(c p) t -> p c t", p=128))
            kb.dma(rope, rope[:, 0, :], I["ropeR"], I["ropeR"].ap[:, sl])
            kb.dma(rope, rope[:, 1, :], I["ropeR"], I["ropeR"].ap[:, T + i * 128:T + (i + 1) * 128])
            kb.tt(V, qk, qk[:, :, :], raw, raw[:, 0:4, :], rope, rope[:, 0:1, :].broadcast_to([128, 4, 128]), ALU.mult)
            kb.tt(V, tmp, tmp[:, :, :], raw, raw[:, 4:8, :], rope, rope[:, 1:2, :].broadcast_to([128, 4, 128]), ALU.mult)
            kb.tt(V, qk, qk[:, :, :], qk, qk[:, :, :], tmp, tmp[:, :, :], ALU.add)
            kb.cp(V, qkb, qkb[:, 0:2, :], qk, qk[:, 0:2, :])
            kb.ts(V, qkb, qkb[:, 2:4, :], qk, qk[:, 2:4, :], 0.125, None, ALU.mult)
            if RCUT < 2:
                return
            pt = PT[0]
            if os.environ.get("RET_VAR") == "c":
                for c in range(4):
                    kb.tr(pt, pt[:, c * 128:(c + 1) * 128], raw, raw[:, 8 + c, :], identb, identb[:, :])
                return
            for c in range(2):
                kb.tr(pt, pt[:, c * 128:(c + 1) * 128], qkb, qkb[:, 2 + c, :], identb, identb[:, :])
            if os.environ.get("RET_VAR") == "b":
                return
            kb.cp(V, ktr, ktr[:, :, :], pt, pt[:, 0:256].rearrange("p (h n) -> p h n", h=4))
            if os.environ.get("RET_VAR") == "a":
                return
            pt2 = PT[1]
            for c in range(4):
                kb.tr(pt2, pt2[:, c * 128:(c + 1) * 128], raw, raw[:, 8 + c, :], identb, identb[:, :])
            kb.cp(S, vtm, vtm[:, :], pt2, pt2[:, 0:512])

        import os
        RCUT = int(os.environ.get("RET_CUT", 100))

        def step(i, d, first):
            load_tile(i)
            if RCUT < 3:
                return
            ps_s, ps_y, ps_r = PS[0], PS[1], PS[2]
            for h in range(4):
                c, o = h // 2, (h % 2) * 64
                pb = ps_s if h % 2 == 0 else PS[3]
                kb.mm(pb, pb[:, c * 128:(c + 1) * 128], qkb, qkb[o:o + 64, 2 + c, :], qkb, qkb[o:o + 64, c, :])
            for h in range(4):
                c = h // 2
                pb = ps_s if h % 2 == 0 else PS[3]
                kb.tt(V, PTt, PTt[:, h, :], pb, pb[:, c * 128:(c + 1) * 128], DM, DM[:, d * 4 + h, :], ALU.mult)
            if RCUT < 4:
                return
            for h in range(4):
                c, o = h // 2, (h % 2) * 64
                dh = d * 4 + h
                kb.tt(V, qx, qx[o:o + 64, c, :], qkb, qkb[o:o + 64, c, :], xi, xi[o:o + 64, dh, :], ALU.mult)
            for h in range(4):
                c, o = h // 2, (h % 2) * 64
                sl = slice(h * 128, (h + 1) * 128)
                kb.mm(ps_y, ps_y[:, sl], PTt, PTt[:, h, :], vtm, vtm[:, sl], True, False)
                kb.mm(ps_y, ps_y[:, sl], qx, qx[o:o + 64, c, :], Rb, Rb[o:o + 64, d * 2 + c, :], False, True)
            if RCUT < 5:
                return
            for h in range(4):
                dh = d * 4 + h
                kb.ts(V, ktm, ktm[:, h, :], ktr, ktr[:, h, :], zt[:, dh:dh + 1], None, ALU.mult, reads=[zt])
            for h in range(4):
                c, o = h // 2, (h % 2) * 64
                kb.mm(ps_r, ps_r[o:o + 64, c * 128:(c + 1) * 128], ktm, ktm[:, h, :], vtm, vtm[:, h * 128:(h + 1) * 128])
            for h in range(4):
                c, o = h // 2, (h % 2) * 64
                dh = d * 4 + h
                kb.stt(R32, R32[o:o + 64, d * 2 + c, :], R32, R32[o:o + 64, d * 2 + c, :], gck[o:o + 64, dh:dh + 1],
                       ps_r, ps_r[o:o + 64, c * 128:(c + 1) * 128], ALU.mult, ALU.add, reads=[gck])
            kb.cp(V, Rb, Rb[:, d * 2:d * 2 + 2, :], R32, R32[:, d * 2:d * 2 + 2, :])
            if RCUT < 6:
                return
            sl = slice(i * 128, (i + 1) * 128)
            if i < 2 and l == n_layers - 1:
                return
            if first:
                kb.cp(S, yt, yt[:, :], ps_y, ps_y[:, :])
                kb.dma(ofw, ofw[sl, :], yt, yt[:, :], q=S)
            else:
                kb.dma(y2, y2[:, :], ofw, ofw[sl, :])
                kb.tt(V, yt, yt[:, :], ps_y, ps_y[:, :], y2, y2[:, :], ALU.add)
                v3 = lambda ap: ap.rearrange("p (h n) -> p h n", h=4)
                kb.red(smr, smr[:, 0:4], yt, v3(yt[:, :]))
                kb.ts(V, smr, smr[:, 0:4], smr, smr[:, 0:4], 1.0 / 128, None, ALU.mult)
                kb.tt(V, yt, v3(yt[:, :]), yt, v3(yt[:, :]), smr, smr[:, 0:4].unsqueeze(2).broadcast_to([128, 4, 128]), ALU.subtract)
                kb.tt(V, ysq, ysq[:, :], yt, yt[:, :], yt, yt[:, :], ALU.mult)
                kb.red(smr, smr[:, 4:8], ysq, v3(ysq[:, :]))
                kb.act(smr, smr[:, 4:8], smr, smr[:, 4:8], AF.Sqrt, scale=1.0 / 128, bias=epsr[:, 2:3], reads=[epsr])
                kb.recip(smr, smr[:, 8:12], smr, smr[:, 4:8])
                kb.tt(V, ybf, v3(ybf[:, :]), yt, v3(yt[:, :]), smr, smr[:, 8:12].unsqueeze(2).broadcast_to([128, 4, 128]), ALU.mult)
                pt = PT[0]
                for c in range(4):
                    kb.tr(pt, pt[:, c * 128:(c + 1) * 128], ybf, ybf[:, c * 128:(c + 1) * 128], identb, identb[:, :])
                kb.dma(gt, gt[:, :, :], uT, uT[1536:2048, sl].rearrange("(c p) t -> p c t", p=128))
                kb.act(gs, gs[:, :, :], gt, gt[:, :, :], AF.Silu)
                for c in range(4):
                    kb.ts(V, y2, y2[:, c * 128:(c + 1) * 128], pt, pt[:, c * 128:(c + 1) * 128], rln[:, c:c + 1], rln[:, 4 + c:5 + c],
                          ALU.mult, ALU.add, reads=[rln])
                kb.tt(V, yo, yo[:, :, :], y2, y2[:, :].rearrange("p (c t) -> p c t", c=4), gs, gs[:, :, :], ALU.mult)
                kb.store(yT, yT[512:1024, sl].rearrange("(c p) t -> p c t", p=128), yo, yo[:, :, :], q=G)

        import os
        nst = int(os.environ.get("RET_STEPS", 100))
        for i in order[0][:nst]:
            step(i, 0, True)
        for i in order[1][:max(0, nst - 34)]:
            step(i, 1, False)

    def phase_mla(l):
        kb.reset()
        SC = 96 ** -0.5
        mn = kb.alloc([128, 5], F32, "mn")
        kb.dma(mn, mn[:, :], I[f"mnorm{l}"], I[f"mnorm{l}"].ap)
        qup = kb.alloc([128, 3, 768], BF16, "qup")
        qsw = kb.alloc([128, 3, 768], BF16, "qsw")
        kvk = kb.alloc([128, 2, 512], BF16, "kvk")
        kvv = kb.alloc([128, 2, 512], BF16, "kvv")
        kb.dma(qup, qup[:, :, :], I[f"qup{l}"], I[f"qup{l}"].ap.rearrange("(c p) n -> p c n", p=128), q=G)
        kb.dma(qsw, qsw[:, :, :], I[f"qupsw{l}"], I[f"qupsw{l}"].ap.rearrange("(c p) n -> p c n", p=128), q=G)
        kb.dma(kvk, kvk[:, :, :], I[f"kvk{l}"], I[f"kvk{l}"].ap.rearrange("(c p) n -> p c n", p=128), q=G)
        kb.dma(kvv, kvv[:, :, :], I[f"kvv{l}"], I[f"kvv{l}"].ap.rearrange("(c p) n -> p c n", p=128), q=G)
        Vaug = kb.alloc([128, NT, 8, 65], BF16, "Vaug")
        kb.memset(V, Vaug, Vaug[:, :, :, :], 1.0)
        lat = kb.alloc([128, 5, 512], BF16, "lat")
        sq = kb.alloc([128, 5, 512], F32, "sq")
        rr = kb.alloc([128, 2, 512], F32, "rr")
        ln = kb.alloc([128, 5, 512], BF16, "ln")
        tA2 = [kb.alloc([96, 512], F32, "tA") for _ in range(2)]
        tB2 = [kb.alloc([96, 512], F32, "tB") for _ in range(2)]
        qh2 = [kb.alloc([96, 512], BF16, "qh") for _ in range(2)]
        rp = kb.alloc([96, 2, 512], BF16, "rp")
        kr = kb.alloc([32, 4, 512], BF16, "kr")
        krf = kb.alloc([32, 2, 512], F32, "krf")
        krb = kb.alloc([32, 512], BF16, "krb")
        kh2 = [kb.alloc([64, 512], BF16, "kh") for _ in range(2)]
        km = kb.alloc([1, 16], F32, "km")
        rowt2 = [kb.alloc([1, 2, 512], F32, "rowt") for _ in range(2)]
        rowb2 = [kb.alloc([1, 512], BF16, "rowb") for _ in range(2)]
        onesb = kb.alloc([1, T], BF16, "onesb")
        kb.memset(V, km, km[:, :], 0.0)
        kb.memset(V, onesb, onesb[:, :], 1.0)
        for h in range(8):
            kb.dma(kaug, kaug[h, 96:97, :], onesb, onesb[:, :])
        qsrc = 2048
        ksrc = 2432
        rsrc = 5760

        def load_norm(tt):
            t0, n = TT512[tt]
            kb.dma(lat, lat[:, :, 0:n], uT, uT[qsrc:qsrc + 640, t0:t0 + n].rearrange("(c p) t -> p c t", p=128))
            kb.tt(V, sq, sq[:, :, 0:n], lat, lat[:, :, 0:n], lat, lat[:, :, 0:n], ALU.mult)
            for j, (c0, c1, dim) in enumerate(((0, 3, 384), (3, 5, 256))):
                ps = PS[j]
                for c in range(c0, c1):
                    kb.mm(ps, ps[:, 0:n], onesf, onesf[:, :], sq, sq[:, c, 0:n], c == c0, c == c1 - 1)
                kb.act(rr, rr[:, j, 0:n], ps, ps[:, 0:n], AF.Sqrt, scale=1.0 / dim, bias=epsr[:, 0:1], reads=[epsr])
                kb.recip(rr, rr[:, j, 0:n], rr, rr[:, j, 0:n])
                for c in range(c0, c1):
                    kb.stt(ln, ln[:, c, 0:n], lat, lat[:, c, 0:n], mn[:, c:c + 1], rr, rr[:, j, 0:n], ALU.mult, ALU.mult, reads=[mn])

        for tt, (t0, n) in enumerate(TT512):
            load_norm(tt)
            for h in range(8):
                ps = PS[2 + h % 2]
                kh, tA, rowt = kh2[h % 2], tA2[h % 2], rowt2[h % 2]
                for c in range(2):
                    kb.mm(ps, ps[0:64, 0:n], kvk, kvk[:, c, h * 64:(h + 1) * 64], ln, ln[:, 3 + c, 0:n], c == 0, c == 1)
                kb.cp(S, kh, kh[:, 0:n], ps, ps[0:64, 0:n])
                kb.store(kaug, kaug[h, 0:64, t0:t0 + n], kh, kh[:, 0:n], q=S)
                kb.tt(V, tA, tA[0:64, 0:n], ps, ps[0:64, 0:n], kh, kh[:, 0:n], ALU.mult)
                pr = PS[4 + h % 2]
                kb.mm(pr, pr[:, 0:n], onesf, onesf[0:64, :], tA, tA[0:64, 0:n])
                kb.red(rowt, rowt[:, 0, 0:1], pr, pr[0:1, 0:n], op=ALU.max)
                kb.tt(V, km, km[:, h:h + 1], km, km[:, h:h + 1], rowt, rowt[:, 0, 0:1], ALU.max)
            kb.dma(kr, kr[0:16, 0, 0:n], uT, uT[rsrc:rsrc + 16, t0:t0 + n])
            kb.dma(kr, kr[16:32, 0, 0:n], uT, uT[rsrc + 16:rsrc + 32, t0:t0 + n])
            kb.dma(kr, kr[0:16, 1, 0:n], uT, uT[rsrc + 16:rsrc + 32, t0:t0 + n])
            kb.dma(kr, kr[16:32, 1, 0:n], uT, uT[rsrc:rsrc + 16, t0:t0 + n])
            kb.dma(kr, kr[:, 2, 0:n], I["ropeK"], I["ropeK"].ap[:, t0:t0 + n])
            kb.dma(kr, kr[:, 3, 0:n], I["ropeK"], I["ropeK"].ap[:, T + t0:T + t0 + n])
            kb.tt(V, krf, krf[:, :, 0:n], kr, kr[:, 0:2, 0:n], kr, kr[:, 2:4, 0:n], ALU.mult)
            kb.tt(V, krf, krf[:, 0, 0:n], krf, krf[:, 0, 0:n], krf, krf[:, 1, 0:n], ALU.add)
            kb.cp(V, krb, krb[:, 0:n], krf, krf[:, 0, 0:n])
            for h in range(8):
                kb.store(kaug, kaug[h, 64:96, t0:t0 + n], krb, krb[:, 0:n], q=G)
            kb.tt(V, krf, krf[:, 1, 0:n], krf, krf[:, 0, 0:n], krf, krf[:, 0, 0:n], ALU.mult)
            pr = PS[4]
            rowt = rowt2[0]
            kb.mm(pr, pr[:, 0:n], onesf, onesf[0:32, :], krf, krf[:, 1, 0:n])
            kb.red(rowt, rowt[:, 0, 0:1], pr, pr[0:1, 0:n], op=ALU.max)
            kb.tt(V, km, km[:, 8:9], km, km[:, 8:9], rowt, rowt[:, 0, 0:1], ALU.max)
            for sub in range(n // 128):
                i = (t0 + sub * 128) // 128
                ps = PS[5]
                for c in range(2):
                    kb.mm(ps, ps[:, :], ln, ln[:, 3 + c, sub * 128:(sub + 1) * 128], kvv, kvv[:, c, :], c == 0, c == 1)
                kb.cp(V, Vaug, Vaug[:, i, :, 0:64], ps, ps[:, :].rearrange("p (h e) -> p h e", h=8))
        kb.ts(V, km, km[:, 0:8], km, km[:, 0:8], km[:, 8:9], None, ALU.add)
        kb.act(km, km[:, 0:8], km, km[:, 0:8], AF.Sqrt)
        kb.ts(V, km, km[:, 0:8], km, km[:, 0:8], -1.0, None, ALU.mult)
        for tt, (t0, n) in enumerate(TT512):
            if tt == 0 and l == n_layers - 1:
                continue
            load_norm(tt)
            kb.dma(rp, rp[:, 0, 0:n], I["ropeM"], I["ropeM"].ap[:, t0:t0 + n])
            kb.dma(rp, rp[:, 1, 0:n], I["ropeM"], I["ropeM"].ap[:, T + t0:T + t0 + n])
            for h in range(8):
                pa, pb = PS[(h % 2) * 2], PS[(h % 2) * 2 + 1]
                tA, tB, qh, rowt, rowb_ = tA2[h % 2], tB2[h % 2], qh2[h % 2], rowt2[h % 2], rowb2[h % 2]
                for c in range(3):
                    kb.mm(pa, pa[0:96, 0:n], qup, qup[:, c, h * 96:(h + 1) * 96], ln, ln[:, c, 0:n], c == 0, c == 2)
                for c in range(3):
                    kb.mm(pb, pb[0:96, 0:n], qsw, qsw[:, c, h * 96:(h + 1) * 96], ln, ln[:, c, 0:n], c == 0, c == 2)
                kb.tt(V, tA, tA[:, 0:n], pa, pa[0:96, 0:n], rp, rp[:, 0, 0:n], ALU.mult)
                kb.tt(V, tB, tB[:, 0:n], pb, pb[0:96, 0:n], rp, rp[:, 1, 0:n], ALU.mult)
                kb.tt(V, tA, tA[:, 0:n], tA, tA[:, 0:n], tB, tB[:, 0:n], ALU.add)
                kb.cp(V, qh, qh[:, 0:n], tA, tA[:, 0:n])
                kb.store(qaug, qaug[h, 0:96, t0:t0 + n], qh, qh[:, 0:n], q=G)
                kb.tt(V, tB, tB[:, 0:n], tA, tA[:, 0:n], tA, tA[:, 0:n], ALU.mult)
                pr = PS[4 + h % 2]
                kb.mm(pr, pr[:, 0:n], onesf, onesf[0:96, :], tB, tB[:, 0:n])
                kb.act(rowt, rowt[:, 1, 0:n], pr, pr[0:1, 0:n], AF.Sqrt)
                kb.ts(V, rowb_, rowb_[:, 0:n], rowt, rowt[:, 1, 0:n], km[:, h:h + 1], None, ALU.mult, reads=[km])
                kb.store(qaug, qaug[h, 96:97, t0:t0 + n], rowb_, rowb_[:, 0:n], q=G)
        P.barrier()
        import os
        if os.environ.get('MLA_SKIP3'):
            return
        ka = kb.alloc([97, T], BF16, "ka")
        qa = [kb.alloc([97, 512], BF16, "qa") for _ in range(2)]
        pT = [kb.alloc([128, 512], BF16, "pT") for _ in range(3)]
        rrow2 = kb.alloc([65, 512], F32, "rrow2")
        bc = kb.alloc([64, 512], F32, "bc")
        yo = kb.alloc([64, 512], BF16, "yo")
        cnt = 0
        qcnt = 0
        pending = []
        for h in range(8):
            kb.dma(ka, ka[:, :], kaug, kaug[h, :, :])
            for tt, (t0, n) in enumerate(TT512):
                if tt == 0 and l == n_layers - 1:
                    continue
                q_ = qa[tt % 2]
                kb.dma(q_, q_[:, 0:n], qaug, qaug[h, :, t0:t0 + n])
                nk = 2 if tt == 0 else NT
                po = PS[2 + qcnt % 2]
                qcnt += 1
                prev = None
                for kt in range(nk):
                    ps = PS[kt % 2]
                    pt_ = pT[cnt % 3]
                    cnt += 1
                    kb.mm(ps, ps[:, 0:n], ka, ka[:, kt * 128:(kt + 1) * 128], q_, q_[:, 0:n])
                    if prev is not None:
                        pk, ppt = prev
                        kb.mm(po, po[0:65, 0:n], Vaug, Vaug[:, pk, h, :], ppt, ppt[:, 0:n], pk == 0, False)
                    kb.act(pt_, pt_[:, 0:n], ps, ps[:, 0:n], AF.Exp, scale=SC)
                    prev = (kt, pt_)
                    if kt == 1 and len(pending) >= 1:
                        pending.pop(0)()
                pk, ppt = prev
                kb.mm(po, po[0:65, 0:n], Vaug, Vaug[:, pk, h, :], ppt, ppt[:, 0:n], pk == 0, True)
                def fin(po=po, n=n, h=h, t0=t0):
                    kb.recip(rrow2, rrow2[64:65, 0:n], po, po[64:65, 0:n])
                    pb = PS[4]
                    kb.mm(pb, pb[:, 0:n], onesf, onesf[64:65, :], rrow2, rrow2[64:65, 0:n])
                    kb.cp(S, bc, bc[:, 0:n], pb, pb[0:64, 0:n])
                    kb.tt(V, yo, yo[:, 0:n], po, po[0:64, 0:n], bc, bc[:, 0:n], ALU.mult)
                    kb.store(yT, yT[1024 + h * 64:1024 + (h + 1) * 64, t0:t0 + n], yo, yo[:, 0:n], q=G)
                pending.append(fin)

        while pending:
            pending.pop(0)()

    def phase_merge(l, hsrc, hdst):
        kb.reset()
        wb = kb.alloc([128, 12, D], BF16, "wb")
        wo = kb.alloc([128, 8, D], BF16, "wo")
        for nb in range(3):
            kb.dma(wb, wb[:, nb * 4:(nb + 1) * 4, :], I[f"wbr{l}"], I[f"wbr{l}"].ap[nb].rearrange("(c p) n -> p c n", p=128), q=G)
        kb.dma(wo, wo[:, :, :], I[f"wout{l}"], I[f"wout{l}"].ap.rearrange("(c p) n -> p c n", p=128), q=G)
        gtb = kb.alloc([128, 2, D], F32, "gtb")
        for w in range(2):
            kb.dma(gtb, gtb[:, w, :].rearrange("p (j q) -> p j q", j=8), modd[l], rowb(l, 16, w))
        yt = kb.alloc([128, 12, 512], BF16, "yt")
        gl = kb.alloc([128, 24, 512], BF16, "gl")
        sg = kb.alloc([128, 512], F32, "sg")
        acc = kb.alloc([128, 512], F32, "acc")
        tm = kb.alloc([128, 512], F32, "tm")
        mT = kb.alloc([128, 8, 512], BF16, "mT")
        hres = [kb.alloc([128, D], F32, "hres") for _ in range(2)]
        hn = [kb.alloc([128, D], F32, "hn") for _ in range(2)]
        cnt = 0
        for tt, (t0, n) in enumerate(TT512):
            if tt == 0 and l == n_layers - 1:
                continue
            kb.dma(yt, yt[:, :, 0:n], yT, yT[:, t0:t0 + n].rearrange("(c p) t -> p c t", p=128))
            kb.dma(gl, gl[:, :, 0:n], uT, uT[2688:5760, t0:t0 + n].rearrange("(c p) t -> p c t", p=128))
            for oc in range(8):
                for nb in range(3):
                    ps = PS[nb]
                    for kc in range(4):
                        kb.mm(ps, ps[:, 0:n], wb, wb[:, nb * 4 + kc, oc * 128:(oc + 1) * 128], yt, yt[:, nb * 4 + kc, 0:n], kc == 0, kc == 3)
                    kb.act(sg, sg[:, 0:n], gl, gl[:, nb * 8 + oc, 0:n], AF.Sigmoid)
                    if nb == 0:
                        kb.tt(V, acc, acc[:, 0:n], ps, ps[:, 0:n], sg, sg[:, 0:n], ALU.mult)
                    else:
                        kb.tt(V, tm, tm[:, 0:n], ps, ps[:, 0:n], sg, sg[:, 0:n], ALU.mult)
                        kb.tt(G, acc, acc[:, 0:n], acc, acc[:, 0:n], tm, tm[:, 0:n], ALU.add)
                kb.cp(V, mT, mT[:, oc, 0:n], acc, acc[:, 0:n])
            w = 1 if tt == 0 else 0
            for sub in range(n // 128):
                tok = t0 + sub * 128
                hr, hn_ = hres[cnt % 2], hn[cnt % 2]
                cnt += 1
                kb.dma(hr, hr[:, :], hsrc, hsrc[tok:tok + 128, :])
                for half in range(2):
                    ps = PS[3 + half]
                    for kc in range(8):
                        kb.mm(ps, ps[:, :], mT, mT[:, kc, sub * 128:(sub + 1) * 128], wo, wo[:, kc, half * 512:(half + 1) * 512], kc == 0, kc == 7)
                    kb.tt(V, hn_, hn_[:, half * 512:(half + 1) * 512], ps, ps[:, :], gtb, gtb[:, w, half * 512:(half + 1) * 512], ALU.mult)
                kb.tt(G, hn_, hn_[:, :], hn_, hn_[:, :], hr, hr[:, :], ALU.add)
                kb.store(hdst, hdst[tok:tok + 128, :], hn_, hn_[:, :], q=G)

    def phase_ffn(l, hsrc, hdst, moe):
        kb.reset()
        vec = MOD[l]
        ncx = NormCtx()
        hm2 = kb.alloc([128, 8, 512], BF16, "hm2")
        rows = kb.alloc([128, 3, D], F32, "rows")
        wg = kb.alloc([128, 8, 1408], BF16, "wg")
        wu = kb.alloc([128, 8, 1408], BF16, "wu")
        wd = kb.alloc([128, 11, D], BF16, "wd")
        hT = kb.alloc([128, 11, 512], BF16, "hT")
        sgl = kb.alloc([128, 512], F32, "sgl")
        hu = kb.alloc([128, 512], F32, "hu")
        acc = kb.alloc([128, 4, D], F32, "acc")
        hr = kb.alloc([128, D], F32, "hr")
        if moe:
            rtb = kb.alloc([128, 8, D], F32, "rtb")
            for e in range(8):
                kb.dma(rtb, rtb[:, e, :], I["routerT"], I["routerT"].ap[e, :].partition_broadcast(128))
            hnk = kb.alloc([128, D], F32, "hnk")
            hmd = kb.alloc([128, D], F32, "hmd")
            jk = kb.alloc([128, D], F32, "jk")
            rs = kb.alloc([128, 64], F32, "rs")
            combT = kb.alloc([8, 512], F32, "combT")
            combb = kb.alloc([128, 8, 512], BF16, "combb")
            sel = kb.alloc([8, 1024], F32, "sel")
            kb.dma(sel, sel[:, :], I["sel8"], I["sel8"].ap)
            experts = [(I["mwg"].ap[e], I["mwu"].ap[e], I["mwd"].ap[e], e) for e in range(8)]
            srcs = (I["mwg"], I["mwu"], I["mwd"])
        else:
            experts = [(I["wg0"].ap[:, hf * 1408:(hf + 1) * 1408], I["wu0"].ap[:, hf * 1408:(hf + 1) * 1408],
                        I["wd0"].ap[hf * 1408:(hf + 1) * 1408, :], None) for hf in range(2)]
            srcs = (I["wg0"], I["wu0"], I["wd0"])
        tiles = list(enumerate(TT512))
        if moe:
            tiles = tiles[1:]
        cur_w = None
        wsc = P.dram(f"wsc{l}", [len(experts), 128, 33792], BF16)
        for ti, (tt, (t0, n)) in enumerate(tiles):
            first_tile = ti == 0
            w = 1 if tt == 0 else 0
            if w != cur_w:
                cur_w = w
                for j, j0 in enumerate((48, 24, 40)):
                    kb.dma(rows, rows[:, j, :].rearrange("p (j q) -> p j q", j=8), modd[l], rowb(l, j0, w))
            nsub = n // 128
            for sub in range(nsub):
                i = (t0 + sub * 128) // 128
                norm_tile(ncx, i, hsrc, vec, 2, hm2, hm2[:, :, sub * 128:(sub + 1) * 128], keep_hn=(hnk if moe else None))
                if moe:
                    kb.tt(V, hmd, hmd[:, :], hnk, hnk[:, :], rows, rows[:, 0, :], ALU.mult)
                    kb.tt(V, hmd, hmd[:, :], hmd, hmd[:, :], rows, rows[:, 1, :], ALU.add)
                    for e in range(8):
                        kb.stt(jk, jk[:, :], hmd, hmd[:, :], 1.0, rtb, rtb[:, e, :], ALU.mult, ALU.mult,
                               accum=rs[:, e:e + 1], writes=[rs])
                    kb.P.op(V, lambda en: en.max(out=rs[:, 8:16], in_=rs[:, 0:8]), reads=[rs], writes=[rs])
                    kb.ts(V, rs, rs[:, 16:24], rs, rs[:, 0:8], rs[:, 9:10], None, ALU.is_ge)
                    kb.ts(V, rs, rs[:, 32:33], rs, rs[:, 8:9], -1.0, None, ALU.mult)
                    kb.act(rs, rs[:, 24:32], rs, rs[:, 0:8], AF.Exp, bias=rs[:, 32:33])
                    kb.tt(V, rs, rs[:, 24:32], rs, rs[:, 24:32], rs, rs[:, 16:24], ALU.mult)
                    kb.red(rs, rs[:, 33:34], rs, rs[:, 24:32])
                    kb.recip(rs, rs[:, 34:35], rs, rs[:, 33:34])
                    kb.ts(V, rs, rs[:, 40:48], rs, rs[:, 24:32], rs[:, 34:35], None, ALU.mult)
                    pc = PS[5]
                    kb.tr(pc, pc[0:8, 0:128], rs, rs[:, 40:48], identf, identf[:, :])
                    kb.cp(V, combT, combT[:, sub * 128:(sub + 1) * 128], pc, pc[0:8, 0:128])
            if moe:
                for e in range(8):
                    pcb = PS[4]
                    kb.mm(pcb, pcb[:, 0:n], sel, sel[:, e * 128:(e + 1) * 128], combT, combT[:, 0:n])
                    kb.cp(V, combb, combb[:, e, 0:n], pcb, pcb[:, 0:n])
            for ei, (ag, au, ad, eidx) in enumerate(experts):
                if first_tile:
                    kb.dma(wg, wg[:, :, :], srcs[0], ag.rearrange("(c p) n -> p c n", p=128), q=G)
                    kb.dma(wu, wu[:, :, :], srcs[1], au.rearrange("(c p) n -> p c n", p=128), q=G)
                    kb.dma(wd, wd[:, :, :], srcs[2], ad.rearrange("(c p) n -> p c n", p=128), q=G)
                    kb.dma(wsc, wsc[ei, :, 0:11264], wg, wg[:, :, :].rearrange("p c n -> p (c n)"))
                    kb.dma(wsc, wsc[ei, :, 11264:22528], wu, wu[:, :, :].rearrange("p c n -> p (c n)"))
                    kb.dma(wsc, wsc[ei, :, 22528:33792], wd, wd[:, :, :].rearrange("p c n -> p (c n)"))
                else:
                    kb.dma(wg, wg[:, :, :].rearrange("p c n -> p (c n)"), wsc, wsc[ei, :, 0:11264])
                    kb.dma(wu, wu[:, :, :].rearrange("p c n -> p (c n)"), wsc, wsc[ei, :, 11264:22528])
                    kb.dma(wd, wd[:, :, :].rearrange("p c n -> p (c n)"), wsc, wsc[ei, :, 22528:33792])
                for fc in range(11):
                    pg, pu = PS[(fc % 2) * 2], PS[(fc % 2) * 2 + 1]
                    for k in range(8):
                        kb.mm(pg, pg[:, 0:n], wg, wg[:, k, fc * 128:(fc + 1) * 128], hm2, hm2[:, k, 0:n], k == 0, k == 7)
                    for k in range(8):
                        kb.mm(pu, pu[:, 0:n], wu, wu[:, k, fc * 128:(fc + 1) * 128], hm2, hm2[:, k, 0:n], k == 0, k == 7)
                    kb.act(sgl, sgl[:, 0:n], pg, pg[:, 0:n], AF.Silu)
                    if eidx is None:
                        kb.tt(V, hT, hT[:, fc, 0:n], pu, pu[:, 0:n], sgl, sgl[:, 0:n], ALU.mult)
                    else:
                        kb.tt(V, hu, hu[:, 0:n], pu, pu[:, 0:n], sgl, sgl[:, 0:n], ALU.mult)
                        kb.tt(G, hT, hT[:, fc, 0:n], hu, hu[:, 0:n], combb, combb[:, eidx, 0:n], ALU.mult)
                for sub in range(nsub):
                    for half in range(2):
                        ps = PS[4 + half]
                        for fc in range(11):
                            kb.mm(ps, ps[:, :], hT, hT[:, fc, sub * 128:(sub + 1) * 128], wd, wd[:, fc, half * 512:(half + 1) * 512], fc == 0, fc == 10)
                        a_ = acc[:, sub, half * 512:(half + 1) * 512]
                        if ei == 0:
                            kb.cp(V, acc, a_, ps, ps[:, :])
                        else:
                            kb.tt(V, acc, a_, ps, ps[:, :], acc, a_, ALU.add)
            for sub in range(nsub):
                tok = t0 + sub * 128
                kb.dma(hr, hr[:, :], hsrc, hsrc[tok:tok + 128, :])
                kb.tt(V, acc, acc[:, sub, :], acc, acc[:, sub, :], rows, rows[:, 2, :], ALU.mult)
                kb.tt(G, acc, acc[:, sub, :], acc, acc[:, sub, :], hr, hr[:, :], ALU.add)
                kb.store(hdst, hdst[tok:tok + 128, :], acc, acc[:, sub, :], q=G)

    def phase_final(hsrc):
        kb.reset()
        fn = kb.alloc([128, D], F32, "fn")
        kb.dma(fn, fn[:, :], I["fnorm"], I["fnorm"].ap[0, :].partition_broadcast(128))
        hts = [kb.alloc([128, D], F32, "ht") for _ in range(2)]
        jk = kb.alloc([128, D], F32, "jk")
        sts = [kb.alloc([128, 4], F32, "st") for _ in range(2)]
        for i in range(2, NT):
            ht, st = hts[i % 2], sts[i % 2]
            kb.dma(ht, ht[:, :], hsrc, hsrc[i * 128:(i + 1) * 128, :])
            kb.act(jk, jk[:, :], ht, ht[:, :], AF.Square, accum=st[:, 0:1], writes=[st])
            kb.act(st, st[:, 1:2], st, st[:, 0:1], AF.Sqrt, scale=1.0 / D, bias=epsr[:, 0:1], reads=[epsr])
            kb.recip(st, st[:, 2:3], st, st[:, 1:2])
            kb.stt(ht, ht[:, :], ht, ht[:, :], st[:, 2:3], fn, fn[:, :], ALU.mult, ALU.mult, reads=[st])
            kb.store(out, out[(i - 2) * 128:(i - 1) * 128, :], ht, ht[:, :], q=G)

    want = set(stop_after) if stop_after is not None else None

    def on(name):
        return want is None or name in want
    for l in range(n_layers):
        if on(f"mod{l}"):
            phase_mod(l)
        else:
            MOD.append(P.sb([128, 4, 8, 2], F32, name=f"vec{l}"))
    hi = 0
    for l in range(n_layers):
        if on(f"norm{l}"):
            kb.reset()
            hm = phase_norm_all(l, hbuf[hi], MOD[l], 0)
            P.barrier()
            mark = hm_mark[0]
            kb.off = mark
            if on(f"rwkv{l}"):
                phase_rwkv_pre(l, hm)
                P.barrier()
            kb.off = mark
            if on(f"proj{l}"):
                phase_proj(l, hm)
        if on(f"rwkv{l}"):
            phase_rwkv(l)
        if on(f"ret{l}"):
            phase_ret(l)
        if on(f"mla{l}"):
            phase_mla(l)
        if on(f"merge{l}"):
            phase_merge(l, hbuf[hi], hbuf[hi + 1])
        if on(f"ffn{l}"):
            phase_ffn(l, hbuf[hi + 1], hbuf[hi + 2], moe=(l % 2 == 1))
        hi += 2
    if on("final"):
        phase_final(hbuf[hi])
    P.emit()
    nc._used_inputs = list(I.keys())
    return nc


def _fm(v, n):
    return np.ascontiguousarray(np.asarray(v, np.float32).reshape(n, 128).T)


def _rope_tables(n_tokens, rot_dim):
    rows = n_tokens // 64
    row = np.repeat(np.arange(rows, dtype=np.float32), 64)
    col = np.tile(np.arange(64, dtype=np.float32), rows)
    n_freq = rot_dim // 4
    inv = np.power(np.float32(10000.0), -np.arange(n_freq, dtype=np.float32) / n_freq).astype(np.float32)
    ang = np.concatenate([row[:, None] * inv, col[:, None] * inv], axis=-1)
    return np.cos(ang).astype(np.float32), np.sin(ang).astype(np.float32)


def _consts():
    bf = ml_dtypes.bfloat16
    c = {}
    c["identb"] = np.eye(128, dtype=np.float32).astype(bf)
    c["identf"] = np.eye(128, dtype=np.float32)
    c["onesf"] = np.ones((128, 128), np.float32)
    s = np.arange(128)[:, None]
    t = np.arange(128)[None, :]
    c["tri"] = np.concatenate([(s <= t), (s >= t)], 1).astype(np.float32)
    fs, fi = (t > s), (t >= s)
    bs, bi = (t < s), (t <= s)
    c["mask4"] = np.concatenate([fs, fi, fs, fi, bs, bi, bs, bi], 1).astype(np.float32).astype(bf)
    c["maskL"] = np.concatenate([(s > t), (s < t)], 1).astype(np.float32).astype(bf)
    blk = lambda b: (s // b) == (t // b)
    c["invmask"] = np.concatenate([blk(2)] + [blk(2 * q) & ~blk(q) for q in (2, 4, 8, 16, 32, 64)], 1).astype(np.float32).astype(bf)
    c["retc"] = np.concatenate([np.abs(t - s) + 0 * s, (t >= s), (t <= s)], 1).astype(np.float32)
    tt = np.arange(128, dtype=np.float32)
    c["rett"] = np.stack([tt + 1, 128 - tt, 127 - tt, tt], 1).astype(np.float32)
    c["retrow"] = np.ascontiguousarray(np.broadcast_to(np.concatenate([tt + 1, 128 - tt])[None, :], (128, 256))).astype(np.float32)
    cs, sn = _rope_tables(NLAT, 64)
    cosR = np.ones((64, T), np.float32)
    sinR = np.zeros((64, T), np.float32)
    cosR[0:32, NCTX:] = cs.T
    cosR[32:64, NCTX:] = cs.T
    sinR[0:32, NCTX:] = -sn.T
    sinR[32:64, NCTX:] = sn.T
    c["ropeR"] = np.concatenate([np.tile(cosR, (2, 1)), np.tile(sinR, (2, 1))], 1).astype(bf)
    cs, sn = _rope_tables(NLAT, 32)
    cosK = np.ones((32, T), np.float32)
    sinK = np.zeros((32, T), np.float32)
    cosK[0:16, NCTX:] = cs.T
    cosK[16:32, NCTX:] = cs.T
    sinK[0:16, NCTX:] = -sn.T
    sinK[16:32, NCTX:] = sn.T
    c["ropeK"] = np.concatenate([cosK, sinK], 1).astype(bf)
    cosE = np.ones((96, T), np.float32)
    sinE = np.zeros((96, T), np.float32)
    cosE[64:96] = cosK
    sinE[64:96] = sinK
    c["ropeM"] = np.concatenate([cosE, sinE], 1).astype(bf)
    sel = np.zeros((8, 8, 128), np.float32)
    for e in range(8):
        sel[e, e, :] = 1.0
    c["sel8"] = sel.reshape(8, 1024)
    return c


def _swap_halves(w, head, half):
    k, n = w.shape
    w3 = w.reshape(k, n // head, 2, half)
    return np.ascontiguousarray(w3[:, :, ::-1, :].reshape(k, n))


def prep_inputs(inp, b, n_layers=2):
    f = lambda a: np.ascontiguousarray(np.asarray(a, np.float32))
    m = dict(_consts())
    m["xin"] = np.concatenate([f(inp["ctx"][b]), f(inp["x"][b])], 0)
    cv = np.stack([_fm(inp["c"][b], 8), _fm(inp["c_ctx"], 8)], -1)
    m["cvec"] = np.ascontiguousarray(cv.reshape(128, 16))
    m["fnorm"] = f(inp["final_norm"]).reshape(1, D)
    for l in range(n_layers):
        m[f"wmod{l}"] = f(inp["w_mod"][l])
        m[f"bmod{l}"] = _fm(inp["b_mod"][l], 48)
        m[f"nmix{l}"] = _fm(inp["norm_mix"][l], 8)
        m[f"nffn{l}"] = _fm(inp["norm_ffn"][l], 8)
        w = f(inp["w_in"][l])
        rw, rt, ml, gt = w[:, 0:1920], w[:, 1920:3456], w[:, 3456:4128], w[:, 4128:7200]
        rq, rk, rv, rg = rt[:, 0:256], rt[:, 256:512], rt[:, 512:1024], rt[:, 1024:1536]
        m[f"win{l}"] = np.ascontiguousarray(np.concatenate(
            [rw, rq, rk, _swap_halves(rq, 64, 32), _swap_halves(rk, 64, 32), rv, rg, ml[:, 0:384], ml[:, 384:640], gt, ml[:, 640:672], np.zeros((D, 96), np.float32)], 1))
        assert m[f"win{l}"].shape[1] == NCOL
        m[f"mu{l}"] = f(inp["rwkv_mu"][l]).reshape(1, 1920)
        m[f"w0{l}"] = f(inp["rwkv_w0"][l])
        m[f"a0{l}"] = f(inp["rwkv_a0"][l])
        m[f"w2{l}"] = f(inp["rwkv_w2"][l]).reshape(128, 512)
        m[f"a2{l}"] = f(inp["rwkv_a2"][l]).reshape(128, 512)
        m[f"g2{l}"] = f(inp["rwkv_g2"][l])
        m[f"rvec{l}"] = np.stack([f(inp[k][l]) for k in ("rwkv_k_k", "rwkv_k_a", "rwkv_r_k", "rwkv_ln_g", "rwkv_ln_b")], 0)
        m[f"rdec{l}"] = f(inp["ret_decay"][l]).reshape(1, 8)
        m[f"rln{l}"] = np.concatenate([_fm(inp["ret_ln_g"][l], 4), _fm(inp["ret_ln_b"][l], 4)], 1)
        m[f"mnorm{l}"] = np.concatenate([_fm(inp["mla_q_norm"][l], 3), _fm(inp["mla_kv_norm"][l], 2)], 1)
        qu = f(inp["mla_q_up"][l])
        m[f"qup{l}"] = qu
        q3 = qu.reshape(384, 8, 96).copy()
        sw = np.zeros_like(q3)
        sw[:, :, 64:80] = q3[:, :, 80:96]
        sw[:, :, 80:96] = q3[:, :, 64:80]
        m[f"qupsw{l}"] = np.ascontiguousarray(sw.reshape(384, 768))
        kv = f(inp["mla_kv_up"][l]).reshape(256, 8, 128)
        m[f"kvk{l}"] = np.ascontiguousarray(kv[:, :, 0:64].reshape(256, 512))
        m[f"kvv{l}"] = np.ascontiguousarray(kv[:, :, 64:128].reshape(256, 512))
        m[f"wbr{l}"] = f(inp["w_branch"][l])
        m[f"wout{l}"] = f(inp["w_out"][l])
    m["wg0"] = f(inp["ffn_w_gate"][0])
    m["wu0"] = f(inp["ffn_w_up"][0])
    m["wd0"] = f(inp["ffn_w_down"][0])
    if n_layers > 1:
        m["routerT"] = np.ascontiguousarray(f(inp["moe_router"][0]).T)
        m["mwg"] = f(inp["moe_w_gate"][0])
        m["mwu"] = f(inp["moe_w_up"][0])
        m["mwd"] = f(inp["moe_w_down"][0])
    return m


_NC_CACHE = {}


def kernel(**inputs):
    if "nc" not in _NC_CACHE:
        _NC_CACHE["nc"] = build()
    nc = _NC_CACHE["nc"]
    in_maps = [prep_inputs(inputs, b) for b in range(8)]
    res = run_bass_kernel_spmd(nc, in_maps, core_ids=list(range(8)))
    return np.stack([np.asarray(r["out"], np.float32) for r in res.results], 0)
```
